# Optimizing a Trainium2 kernel written in Bass

```python
import math
import numpy as np
import jax, jax.numpy as jnp
from jax import lax

D_MODEL = 1024
BATCH = 8
SEQ = 4096
DEPTH = 1

HEAD_DIM = 64
ROT_DIM = HEAD_DIM // 4
ROPE_THETA = 500000.0
NORM_EPS = 1e-6
NEG_INF = -1e30
BIG = 1e30

A_HEADS = 8
A_BLOCK = 256
A_TOPK = 3
A_QBLOCK = 64

B_HEADS = 8
B_KV_HEADS = 2
B_GROUP = B_HEADS // B_KV_HEADS
CMP_LEN = 32
CMP_STRIDE = 16
CMP_HIDDEN = 256
SLC_BLOCK = 64
SLC_TOPN = 16
WINDOW = 512
B_QBLOCK = 64

PEER_HEADS = 8
PEER_NKEYS = 128
PEER_N = PEER_NKEYS * PEER_NKEYS
PEER_QDIM = 256
PEER_HALF = PEER_QDIM // 2
PEER_TOPK = 16
PEER_CHUNK = 128

A_WIDTH = A_HEADS * HEAD_DIM
B_WIDTH = B_HEADS * HEAD_DIM
B_KV_WIDTH = B_KV_HEADS * HEAD_DIM
N_BRANCH = 2
IN_COLS = 3 * A_WIDTH + B_WIDTH + 6 * B_KV_WIDTH + 3 * B_HEADS + N_BRANCH * D_MODEL

kernel_name = "hybrid_moba_nsa_peer_block"


def rmsnorm(x, g):
    xf = x.astype(jnp.float32)
    y = xf * lax.rsqrt(jnp.mean(xf * xf, axis=-1, keepdims=True) + NORM_EPS)
    return (y * g.astype(jnp.float32)).astype(x.dtype)


def rope_tables(S):
    inv = ROPE_THETA ** (-jnp.arange(0, ROT_DIM, 2, dtype=jnp.float32) / ROT_DIM)
    ang = jnp.arange(S, dtype=jnp.float32)[:, None] * inv[None, :]
    return jnp.cos(ang), jnp.sin(ang)


def partial_rope(x, cos, sin):
    c = cos[None, :, None, :].astype(x.dtype)
    s = sin[None, :, None, :].astype(x.dtype)
    x1 = x[..., :ROT_DIM // 2]
    x2 = x[..., ROT_DIM // 2:ROT_DIM]
    return jnp.concatenate([x1 * c - x2 * s, x2 * c + x1 * s, x[..., ROT_DIM:]], axis=-1)


def masked_softmax(s, mask):
    p = jax.nn.softmax(jnp.where(mask, s.astype(jnp.float32), NEG_INF), axis=-1)
    return p * jnp.any(mask, axis=-1, keepdims=True)


def split_columns(t):
    sizes = (A_WIDTH, A_WIDTH, A_WIDTH, B_WIDTH, B_KV_WIDTH, B_KV_WIDTH, B_KV_WIDTH,
             B_KV_WIDTH, B_KV_WIDTH, B_KV_WIDTH, 3 * B_HEADS, N_BRANCH * D_MODEL)
    points, acc = [], 0
    for s in sizes[:-1]:
        acc += s
        points.append(acc)
    return jnp.split(t, points, axis=-1)


def moba_attention(q, k, v):
    B, S, H, D = q.shape
    n_blk = -(-S // A_BLOCK)
    pad = n_blk * A_BLOCK - S
    topk = min(A_TOPK, n_blk)
    nc = S // A_QBLOCK
    Q = A_QBLOCK
    L = A_BLOCK
    scale = HEAD_DIM ** -0.5

    def to_blocks(t):
        t = jnp.pad(t, ((0, 0), (0, pad), (0, 0), (0, 0)))
        return t.reshape(B, n_blk, L, H, D).transpose(0, 3, 1, 2, 4)

    kb, vb = to_blocks(k), to_blocks(v)
    qh = q.transpose(0, 2, 1, 3)
    k_mean = jnp.mean(kb.astype(jnp.float32), axis=3)
    gate = jnp.einsum('bhsd,bhnd->bhsn', qh.astype(jnp.float32), k_mean)
    own = jnp.arange(S) // A_BLOCK
    past = jnp.arange(n_blk)[None, :] < own[:, None]
    gate = jnp.where(past, gate, NEG_INF)
    top_v, sel = lax.top_k(gate, topk)
    valid = top_v > 0.5 * NEG_INF

    def per_batch(args):
        q_b, kb_b, vb_b, sel_b, valid_b = args

        def per_chunk(c):
            start = c * Q
            pos = start + jnp.arange(Q)
            qc = lax.dynamic_slice_in_dim(q_b, start, Q, axis=1)
            sc = lax.dynamic_slice_in_dim(sel_b, start, Q, axis=1)
            vc = lax.dynamic_slice_in_dim(valid_b, start, Q, axis=1)
            kg = jax.vmap(lambda kbh, ih: kbh[ih])(kb_b, sc)
            vg = jax.vmap(lambda vbh, ih: vbh[ih])(vb_b, sc)
            blk = start // A_BLOCK
            k_own = lax.dynamic_index_in_dim(kb_b, blk, axis=1, keepdims=False)
            v_own = lax.dynamic_index_in_dim(vb_b, blk, axis=1, keepdims=False)
            s_sel = jnp.einsum('hqd,hqkld->hqkl', qc, kg).reshape(H, Q, topk * L)
            s_own = jnp.einsum('hqd,hld->hql', qc, k_own)
            m_sel = jnp.broadcast_to(vc[..., None], (H, Q, topk, L)).reshape(H, Q, topk * L)
            kpos = blk * A_BLOCK + jnp.arange(L)
            m_own = jnp.broadcast_to(kpos[None, None, :] <= pos[None, :, None], (H, Q, L))
            s = jnp.concatenate([s_sel, s_own], axis=-1).astype(jnp.float32) * scale
            p = masked_softmax(s, jnp.concatenate([m_sel, m_own], axis=-1)).astype(v.dtype)
            o = jnp.einsum('hqkl,hqkld->hqd', p[..., :topk * L].reshape(H, Q, topk, L), vg)
            return o + jnp.einsum('hql,hld->hqd', p[..., topk * L:], v_own)

        outs = lax.map(per_chunk, jnp.arange(nc))
        return outs.transpose(0, 2, 1, 3).reshape(S, H, D)

    return lax.map(per_batch, (qh, kb, vb, sel, valid))


def compress(t, pos_emb, w1, b1, w2, b2):
    B, S, G, D = t.shape
    n_cmp = (S - CMP_LEN) // CMP_STRIDE + 1
    idx = np.arange(n_cmp)[:, None] * CMP_STRIDE + np.arange(CMP_LEN)[None, :]
    blocks = t[:, idx] + pos_emb[None, None, :, None, :]
    flat = blocks.transpose(0, 3, 1, 2, 4).reshape(B, G, n_cmp, CMP_LEN * D)
    return jax.nn.gelu(flat @ w1 + b1) @ w2 + b2


def nsa_attention(q, kc, vc, ks, vs, kw, vw, gates):
    B, S, H, D = q.shape
    G, R, Q = B_KV_HEADS, B_GROUP, B_QBLOCK
    n_cmp = kc.shape[2]
    n_slc = S // SLC_BLOCK
    topn = min(SLC_TOPN, n_slc)
    nc = S // Q
    scale = HEAD_DIM ** -0.5
    cmp_end = jnp.arange(n_cmp) * CMP_STRIDE + (CMP_LEN - 1)
    ci = np.arange(n_cmp)[:, None]
    sj = np.arange(n_slc)[None, :]
    overlap = jnp.asarray(((ci * CMP_STRIDE < (sj + 1) * SLC_BLOCK) &
                           (ci * CMP_STRIDE + CMP_LEN > sj * SLC_BLOCK)).astype(np.float32))

    qg = q.reshape(B, S, G, R, D).transpose(0, 2, 3, 1, 4)
    ksb = ks.transpose(0, 2, 1, 3).reshape(B, G, n_slc, SLC_BLOCK, D)
    vsb = vs.transpose(0, 2, 1, 3).reshape(B, G, n_slc, SLC_BLOCK, D)
    kwp = jnp.pad(kw.transpose(0, 2, 1, 3), ((0, 0), (0, 0), (WINDOW, 0), (0, 0)))
    vwp = jnp.pad(vw.transpose(0, 2, 1, 3), ((0, 0), (0, 0), (WINDOW, 0), (0, 0)))
    gg = gates.reshape(B, S, G, R, 3).transpose(0, 2, 3, 1, 4)

    def per_batch(args):
        q_b, kc_b, vc_b, ks_b, vs_b, kw_b, vw_b, g_b = args

        def per_chunk(c):
            start = c * Q
            pos = start + jnp.arange(Q)
            qc = lax.dynamic_slice_in_dim(q_b, start, Q, axis=2)
            gc = lax.dynamic_slice_in_dim(g_b, start, Q, axis=2)
            s = jnp.einsum('grqd,gnd->grqn', qc, kc_b).astype(jnp.float32) * scale
            p_cmp = masked_softmax(s, cmp_end[None, :] <= pos[:, None])
            o_cmp = jnp.einsum('grqn,gnd->grqd', p_cmp.astype(vc_b.dtype), vc_b)
            imp = jnp.einsum('grqn,nj->gqj', p_cmp, overlap)
            own = pos // SLC_BLOCK
            blk = jnp.arange(n_slc)[None, :]
            forced = (blk == 0) | (blk == own[:, None]) | (blk == own[:, None] - 1)
            imp = jnp.where(forced, BIG, imp)
            imp = jnp.where(blk <= own[:, None], imp, NEG_INF)
            top_v, sel = lax.top_k(imp, topn)
            valid = top_v > 0.5 * NEG_INF
            kg = jax.vmap(lambda kbg, ig: kbg[ig])(ks_b, sel)
            vg = jax.vmap(lambda vbg, ig: vbg[ig])(vs_b, sel)
            s = jnp.einsum('grqd,gqnld->grqnl', qc, kg).astype(jnp.float32) * scale
            kpos = sel[..., None] * SLC_BLOCK + jnp.arange(SLC_BLOCK)
            m = valid[..., None] & (kpos <= pos[None, :, None, None])
            p = masked_softmax(s.reshape(G, R, Q, topn * SLC_BLOCK),
                               m.reshape(G, 1, Q, topn * SLC_BLOCK))
            o_slc = jnp.einsum('grqnl,gqnld->grqd',
                               p.reshape(G, R, Q, topn, SLC_BLOCK).astype(vs_b.dtype), vg)
            kwc = lax.dynamic_slice_in_dim(kw_b, start, WINDOW + Q, axis=1)
            vwc = lax.dynamic_slice_in_dim(vw_b, start, WINDOW + Q, axis=1)
            kpos_w = start - WINDOW + jnp.arange(WINDOW + Q)
            m_w = ((kpos_w[None, :] <= pos[:, None]) & (kpos_w[None, :] > pos[:, None] - WINDOW)
                   & (kpos_w[None, :] >= 0))
            s = jnp.einsum('grqd,gkd->grqk', qc, kwc).astype(jnp.float32) * scale
            p = masked_softmax(s, m_w)
            o_win = jnp.einsum('grqk,gkd->grqd', p.astype(vwc.dtype), vwc)
            return gc[..., 0:1] * o_cmp + gc[..., 1:2] * o_slc + gc[..., 2:3] * o_win

        outs = lax.map(per_chunk, jnp.arange(nc))
        return outs.transpose(0, 3, 1, 2, 4).reshape(S, H, D)

    return lax.map(per_batch, (qg, kc, vc, ksb, vsb, kwp, vwp, gg))


def peer_ffn(h, w_q, sub_k1, sub_k2, expert_u, expert_v):
    B, S, Dm = h.shape
    T = B * S
    ht = h.reshape(T, Dm)
    q = (ht @ w_q).reshape(T, PEER_HEADS, PEER_QDIM)
    s1 = jnp.einsum('thd,nd->thn', q[..., :PEER_HALF], sub_k1).astype(jnp.float32)
    s2 = jnp.einsum('thd,nd->thn', q[..., PEER_HALF:], sub_k2).astype(jnp.float32)
    v1, i1 = lax.top_k(s1, PEER_TOPK)
    v2, i2 = lax.top_k(s2, PEER_TOPK)
    cand = (v1[..., :, None] + v2[..., None, :]).reshape(T, PEER_HEADS, PEER_TOPK * PEER_TOPK)
    cand_idx = (i1[..., :, None] * PEER_NKEYS + i2[..., None, :]).reshape(T, PEER_HEADS, PEER_TOPK * PEER_TOPK)
    vals, pos = lax.top_k(cand, PEER_TOPK)
    experts = jnp.take_along_axis(cand_idx, pos, axis=-1)
    gates = jax.nn.softmax(vals, axis=-1)

    def per_chunk(args):
        xc, ec, gc = args
        u = expert_u[ec]
        a = jnp.einsum('cd,chkd->chk', xc, u).astype(jnp.float32)
        w = (jax.nn.gelu(a) * gc).astype(xc.dtype)
        return jnp.einsum('chk,chkd->cd', w, expert_v[ec])

    nc = T // PEER_CHUNK
    out = lax.map(per_chunk, (ht.reshape(nc, PEER_CHUNK, Dm),
                              experts.reshape(nc, PEER_CHUNK, PEER_HEADS, PEER_TOPK),
                              gates.reshape(nc, PEER_CHUNK, PEER_HEADS, PEER_TOPK)))
    return out.reshape(B, S, Dm)


def hybrid_layer(x, cos, sin, norm1_g, w_in, b_merge, a_q_g, a_k_g, b_q_g, b_kc_g, b_ks_g, b_kw_g,
                 cmp_pos_k, cmp_k_w1, cmp_k_b1, cmp_k_w2, cmp_k_b2,
                 cmp_pos_v, cmp_v_w1, cmp_v_b1, cmp_v_w2, cmp_v_b2,
                 w_up_a, w_up_b, w_out, norm2_g, peer_wq, peer_k1, peer_k2, peer_u, peer_v):
    B, S, _ = x.shape
    h = rmsnorm(x, norm1_g)
    (aq, ak, av, bq, bkc, bvc, bks, bvs, bkw, bvw, bgate, mgate) = split_columns(h @ w_in)

    def heads(t, n):
        return t.reshape(B, S, n, HEAD_DIM)

    qa = partial_rope(rmsnorm(heads(aq, A_HEADS), a_q_g), cos, sin)
    ka = partial_rope(rmsnorm(heads(ak, A_HEADS), a_k_g), cos, sin)
    ya = moba_attention(qa, ka, heads(av, A_HEADS)).reshape(B, S, A_WIDTH)

    qb = partial_rope(rmsnorm(heads(bq, B_HEADS), b_q_g), cos, sin)
    kc = rmsnorm(compress(heads(bkc, B_KV_HEADS), cmp_pos_k, cmp_k_w1, cmp_k_b1, cmp_k_w2, cmp_k_b2), b_kc_g)
    vc = compress(heads(bvc, B_KV_HEADS), cmp_pos_v, cmp_v_w1, cmp_v_b1, cmp_v_w2, cmp_v_b2)
    ks = partial_rope(rmsnorm(heads(bks, B_KV_HEADS), b_ks_g), cos, sin)
    kw = partial_rope(rmsnorm(heads(bkw, B_KV_HEADS), b_kw_g), cos, sin)
    gb = jax.nn.sigmoid(bgate.astype(jnp.float32)).astype(x.dtype).reshape(B, S, B_HEADS, 3)
    yb = nsa_attention(qb, kc, vc, ks, heads(bvs, B_KV_HEADS), kw, heads(bvw, B_KV_HEADS), gb)
    yb = yb.reshape(B, S, B_WIDTH)

    g = jax.nn.sigmoid((mgate.reshape(B, S, N_BRANCH, D_MODEL) + b_merge).astype(jnp.float32)).astype(x.dtype)
    merged = g[:, :, 0] * (ya @ w_up_a) + g[:, :, 1] * (yb @ w_up_b)
    x = x + merged @ w_out

    return x + peer_ffn(rmsnorm(x, norm2_g), peer_wq, peer_k1, peer_k2, peer_u, peer_v)


def setup_inputs(seed: int = 0) -> dict:
    key = jax.random.key(seed)
    ks = jax.random.split(key, 32)
    L = DEPTH
    f32 = jnp.float32

    def nrm(k, shape, scale):
        return jax.random.normal(k, shape, f32) * scale

    def gain(k, shape):
        return 1.0 + 0.05 * jax.random.normal(k, shape, f32)

    return {
        "x": nrm(ks[0], (BATCH, SEQ, D_MODEL), 1.0),
        "norm1_g": gain(ks[1], (L, D_MODEL)),
        "w_in": nrm(ks[2], (L, D_MODEL, IN_COLS), D_MODEL ** -0.5),
        "b_merge": nrm(ks[3], (L, N_BRANCH, D_MODEL), 0.1),
        "a_q_g": gain(ks[4], (L, HEAD_DIM)),
        "a_k_g": gain(ks[5], (L, HEAD_DIM)),
        "b_q_g": gain(ks[6], (L, HEAD_DIM)),
        "b_kc_g": gain(ks[7], (L, HEAD_DIM)),
        "b_ks_g": gain(ks[8], (L, HEAD_DIM)),
        "b_kw_g": gain(ks[9], (L, HEAD_DIM)),
        "cmp_pos_k": nrm(ks[10], (L, CMP_LEN, HEAD_DIM), 0.1),
        "cmp_k_w1": nrm(ks[11], (L, CMP_LEN * HEAD_DIM, CMP_HIDDEN), (CMP_LEN * HEAD_DIM) ** -0.5),
        "cmp_k_b1": nrm(ks[12], (L, CMP_HIDDEN), 0.02),
        "cmp_k_w2": nrm(ks[13], (L, CMP_HIDDEN, HEAD_DIM), CMP_HIDDEN ** -0.5),
        "cmp_k_b2": nrm(ks[14], (L, HEAD_DIM), 0.02),
        "cmp_pos_v": nrm(ks[15], (L, CMP_LEN, HEAD_DIM), 0.1),
        "cmp_v_w1": nrm(ks[16], (L, CMP_LEN * HEAD_DIM, CMP_HIDDEN), (CMP_LEN * HEAD_DIM) ** -0.5),
        "cmp_v_b1": nrm(ks[17], (L, CMP_HIDDEN), 0.02),
        "cmp_v_w2": nrm(ks[18], (L, CMP_HIDDEN, HEAD_DIM), CMP_HIDDEN ** -0.5),
        "cmp_v_b2": nrm(ks[19], (L, HEAD_DIM), 0.02),
        "w_up_a": nrm(ks[20], (L, A_WIDTH, D_MODEL), A_WIDTH ** -0.5),
        "w_up_b": nrm(ks[21], (L, B_WIDTH, D_MODEL), B_WIDTH ** -0.5),
        "w_out": nrm(ks[22], (L, D_MODEL, D_MODEL), D_MODEL ** -0.5),
        "norm2_g": gain(ks[23], (L, D_MODEL)),
        "peer_wq": nrm(ks[24], (L, D_MODEL, PEER_HEADS * PEER_QDIM), D_MODEL ** -0.5),
        "peer_k1": nrm(ks[25], (L, PEER_NKEYS, PEER_HALF), PEER_HALF ** -0.5),
        "peer_k2": nrm(ks[26], (L, PEER_NKEYS, PEER_HALF), PEER_HALF ** -0.5),
        "peer_u": nrm(ks[27], (L, PEER_N, D_MODEL), D_MODEL ** -0.5),
        "peer_v": nrm(ks[28], (L, PEER_N, D_MODEL), 0.3),
    }


def reference(x, norm1_g, w_in, b_merge, a_q_g, a_k_g, b_q_g, b_kc_g, b_ks_g, b_kw_g,
              cmp_pos_k, cmp_k_w1, cmp_k_b1, cmp_k_w2, cmp_k_b2,
              cmp_pos_v, cmp_v_w1, cmp_v_b1, cmp_v_w2, cmp_v_b2,
              w_up_a, w_up_b, w_out, norm2_g, peer_wq, peer_k1, peer_k2, peer_u, peer_v):
    cos, sin = rope_tables(x.shape[1])
    for l in range(DEPTH):
        x = hybrid_layer(x, cos, sin, norm1_g[l], w_in[l], b_merge[l], a_q_g[l], a_k_g[l],
                         b_q_g[l], b_kc_g[l], b_ks_g[l], b_kw_g[l],
                         cmp_pos_k[l], cmp_k_w1[l], cmp_k_b1[l], cmp_k_w2[l], cmp_k_b2[l],
                         cmp_pos_v[l], cmp_v_w1[l], cmp_v_b1[l], cmp_v_w2[l], cmp_v_b2[l],
                         w_up_a[l], w_up_b[l], w_out[l], norm2_g[l],
                         peer_wq[l], peer_k1[l], peer_k2[l], peer_u[l], peer_v[l])
    return x
```

```python
from contextlib import ExitStack
import math
import numpy as np
import concourse.bass as bass
import concourse.mybir as mybir
from concourse.bass_utils import run_bass_kernel_spmd

F32 = mybir.dt.float32
BF16 = mybir.dt.bfloat16
ALU = mybir.AluOpType
AF = mybir.ActivationFunctionType
AX = mybir.AxisListType

S_LEN = 4096
NT = 32
NT_RUN = 32
SKIP = set()
QT_LIST = None
PEER_GROUPS = None
DM = 1024
NEG = -30000.0
BIGF = 1e30
EPS = 1e-6
SCALE = 0.125

ENGS = ("pe", "act", "dve", "pool", "sp")
EPOCH = 20000
NDMA = {"sp": 16, "act": 8, "pool": 12}


class _Op:
    __slots__ = ("eng", "fn", "dma", "gid", "deps", "signal", "sem", "val", "pre", "drain")

    def __init__(self, eng, fn, dma):
        self.eng = eng
        self.fn = fn
        self.dma = dma
        self.deps = ()
        self.signal = False
        self.sem = None
        self.val = 0
        self.pre = None
        self.drain = False


class Sched:
    def __init__(self):
        self.ops = []
        self.lastw = {}
        self.readers = {}
        self.last_op = {}
        self.excl = set()

    def add(self, eng, fn, reads=(), writes=(), dma=False):
        if self.excl:
            ex = [t for t in reads if t in self.excl]
            if ex:
                writes = list(writes) + [t for t in ex if t not in writes]
        op = _Op(eng, fn, dma)
        op.gid = len(self.ops)
        ops = self.ops
        keep = set()

        def consider(kind, d):
            dop = ops[d]
            if dop.dma:
                keep.add(d)
                return
            if dop.eng == eng and not dma and eng == "pe":
                return
            keep.add(d)

        for t in reads:
            w = self.lastw.get(t)
            if w is not None:
                consider("raw", w)
        for t in writes:
            w = self.lastw.get(t)
            if w is not None:
                consider("waw", w)
            rd = self.readers.get(t)
            if rd:
                for r in rd.values():
                    for g in r:
                        consider("war", g)
        op.deps = keep
        for t in reads:
            rd = self.readers.setdefault(t, {})
            if dma:
                rd.setdefault("dma", []).append(op.gid)
            else:
                rd[eng] = [op.gid]
        for t in writes:
            self.lastw[t] = op.gid
            self.readers[t] = {}
        if not dma:
            self.last_op[eng] = op.gid
        ops.append(op)
        return op

    def barrier(self):
        a_ids = []
        for e in ENGS:
            op = self.add(e, lambda h: h.nop(), writes=[("barA", e)])
            op.drain = True
            lo = None
            for g in range(op.gid - 1, -1, -1):
                o = self.ops[g]
                if o.eng == e and not o.dma:
                    lo = g
                    break
            if lo is not None:
                op.deps = set(op.deps) | {lo}
            a_ids.append(op.gid)
        for e in ENGS:
            op = self.add(e, lambda h: h.nop(), writes=[("barB", e)])
            op.deps = set(op.deps) | set(a_ids)
        self.lastw.clear()
        self.readers.clear()

    def emit(self, nc, enter):
        ops = self.ops
        for op in ops:
            for d in op.deps:
                ops[d].signal = True
        ncnt = {e: 0 for e in ENGS}
        ndma = {e: 0 for e in ENGS}
        for op in ops:
            if op.dma:
                ndma[op.eng] += 1
            elif op.signal:
                ncnt[op.eng] += 1
        sems = {}
        for e in ENGS:
            n_ep = max(1, (ncnt[e] + EPOCH - 1) // EPOCH)
            sems[e] = [enter(nc.semaphore(f"s_{e}{k}")) for k in range(n_ep)]
        dsems = {}
        for e in ENGS:
            if ndma[e]:
                dsems[e] = [enter(nc.semaphore(f"d_{e}{k}")) for k in range(min(NDMA.get(e, 8), ndma[e]))]
        cnt = {e: 0 for e in ENGS}
        dcnt = {e: 0 for e in ENGS}
        for op in ops:
            e = op.eng
            if op.dma:
                i = dcnt[e]
                dcnt[e] += 1
                n = len(dsems[e])
                op.sem = dsems[e][i % n]
                op.val = 16 * (i // n + 1)
                op.pre = (op.sem, 16 * (i // n)) if i >= n else None
            elif op.signal:
                k = cnt[e]
                cnt[e] += 1
                op.sem = sems[e][k // EPOCH]
                op.val = k % EPOCH + 1
        per_eng = {e: [op for op in ops if op.eng == e] for e in ENGS}

        def run_engine(ename, handle, final=False):
            waited = {}
            outstanding = {}

            def wait(sem, val):
                key = id(sem)
                if waited.get(key, 0) < val:
                    handle.wait_ge(sem, val)
                    waited[key] = val

            for op in per_eng[ename]:
                for d in sorted(op.deps):
                    dop = ops[d]
                    wait(dop.sem, dop.val)
                if op.pre is not None:
                    wait(*op.pre)
                if op.drain:
                    for (sem, val) in outstanding.values():
                        wait(sem, val)
                inst = op.fn(handle)
                if op.dma:
                    inst.then_inc(op.sem, 16)
                    outstanding[id(op.sem)] = (op.sem, op.val)
                elif op.signal:
                    inst.then_inc(op.sem, 1)
            if final:
                for (sem, val) in outstanding.values():
                    wait(sem, val)

        return run_engine


class Builder:
    def __init__(self, nc, dbg=False):
        self.nc = nc
        self.S = Sched()
        self.dbg = dbg
        self.dbg_outs = []
        self.root = ExitStack()
        self.stack = self.root
        self._uid = 0

    def sb(self, name, shape, dt=F32):
        self._uid += 1
        return self.stack.enter_context(self.nc.sbuf_tensor(f"{name}_{self._uid}", list(shape), dt))

    def ps(self, name, shape, dt=F32, tok=None):
        self._uid += 1
        nbytes = int(np.prod(shape[1:])) * (4 if dt == F32 else 2)
        assert nbytes == 2048, (name, shape)
        self.S.excl.add(tok if tok is not None else name)
        return self.stack.enter_context(self.nc.psum_tensor(f"{name}_{self._uid}", list(shape), dt))

    def scope(self):
        b = self

        class _Sc:
            def __enter__(s):
                s.prev = b.stack
                s.st = ExitStack()
                b.stack = s.st
                return s

            def __exit__(s, *a):
                b.S.barrier()
                s.st.close()
                b.stack = s.prev
                return False

        return _Sc()

    def mm(self, out, lhsT, rhs, start, stop, r, w):
        self.S.add("pe", lambda e: e.matmul(out, lhsT=lhsT, rhs=rhs, start=start, stop=stop), r, w)

    def tr(self, out, in_, ident, r, w):
        self.S.add("pe", lambda e: e.transpose(out=out, in_=in_, identity=ident), r, w)

    def act(self, out, in_, func, r, w, bias=None, scale=None, accum=None):
        kw = {}
        if bias is not None:
            kw["bias"] = bias
        if scale is not None:
            kw["scale"] = scale
        if accum is not None:
            kw["accum_out"] = accum
        self.S.add("act", lambda e: e.activation(out=out, in_=in_, func=func, **kw), r, w)

    def tt(self, eng, out, a, b, op, r, w):
        self.S.add(eng, lambda e: e.tensor_tensor(out=out, in0=a, in1=b, op=op), r, w)

    def ts(self, eng, out, a, s1, s2, op0, op1, r, w):
        if op1 is None:
            self.S.add(eng, lambda e: e.tensor_scalar(out=out, in0=a, scalar1=s1, scalar2=None, op0=op0), r, w)
        else:
            self.S.add(eng, lambda e: e.tensor_scalar(out=out, in0=a, scalar1=s1, scalar2=s2, op0=op0, op1=op1), r, w)

    def stt(self, eng, out, a, scalar, b, op0, op1, r, w):
        self.S.add(eng, lambda e: e.scalar_tensor_tensor(out=out, in0=a, scalar=scalar, in1=b, op0=op0, op1=op1), r, w)

    def cp(self, eng, out, in_, r, w):
        if eng == "act":
            self.S.add("act", lambda e: e.copy(out=out, in_=in_), r, w)
        else:
            self.S.add(eng, lambda e: e.tensor_copy(out=out, in_=in_), r, w)

    def red(self, out, in_, op, r, w, eng="dve"):
        self.S.add(eng, lambda e: e.tensor_reduce(out=out, in_=in_, axis=AX.X, op=op), r, w)

    def recip(self, out, in_, r, w):
        self.S.add("dve", lambda e: e.reciprocal(out=out, in_=in_), r, w)

    def memset(self, eng, ap, val, w):
        self.S.add(eng, lambda e: e.memset(ap, val), (), w)

    def asel(self, out, in_, pattern, cmp, fill, base, cm, r, w):
        self.S.add("pool", lambda e: e.affine_select(out=out, in_=in_, pattern=pattern, compare_op=cmp, fill=fill,
                                                     base=base, channel_multiplier=cm), r, w)

    def dma(self, eng, out, in_, r, w):
        self.S.add(eng, lambda e: e.dma_start(out=out, in_=in_), r, w, dma=True)

    def max8(self, out, in_, r, w):
        self.S.add("dve", lambda e: e.max(out=out, in_=in_), r, w)

    def mrep(self, out, rep, vals, imm, r, w):
        self.S.add("dve", lambda e: e.match_replace(out=out, in_to_replace=rep, in_values=vals, imm_value=imm), r, w)

    def dump(self, name, ap, tok, shape, dt=F32):
        if not self.dbg:
            return
        d = self.nc.dram_tensor("dbg_" + name, list(shape), dt, kind="ExternalOutput")
        self.dbg_outs.append("dbg_" + name)
        self.dma("sp", d.ap(), ap, [tok] if not isinstance(tok, list) else tok, [("dbgo", name)])


def bcast_rows(t, n, width, off=0):
    return bass.AP(t, off, [[0, n], [1, width]])


class StopBuild(Exception):
    pass


def build_program(dbg=False, stop_after=None):
    nc = bass.Bass("TRN2", target_bir_lowering=False)
    B = Builder(nc, dbg)
    try:
        _build(nc, B, dbg, stop_after)
    except StopBuild:
        pass
    return nc, B


def _build(nc, B, dbg, stop_after):
    def stop(name):
        if stop_after == name:
            raise StopBuild()

    S = B.S
    D = {}

    def din(name, shape):
        D[name] = nc.dram_tensor(name, list(shape), F32, kind="ExternalInput")
        return D[name]

    x_d = din("x", [S_LEN, DM])
    wa_d = din("w_a", [DM, 1536])
    n1g_d = din("norm1_g", [1, DM])
    aqg_d = din("a_q_g", [1, 64])
    akg_d = din("a_k_g", [1, 64])
    wb_d = din("w_b", [DM, 1304])
    wm_d = din("w_m", [DM, 2048])
    bqg_d = din("b_q_g", [1, 64])
    bkcg_d = din("b_kc_g", [1, 64])
    bksg_d = din("b_ks_g", [1, 64])
    bkwg_d = din("b_kw_g", [1, 64])
    cmp_d = {}
    for kv in ("k", "v"):
        cmp_d[kv] = dict(
            posT=din(f"cmp_pos_{kv}T", [64, 64]), w1=din(f"cmp_{kv}_w1", [2048, 256]),
            b1=din(f"cmp_{kv}_b1", [128, 2]), w2=din(f"cmp_{kv}_w2", [256, 64]), b2=din(f"cmp_{kv}_b2", [1, 64]))
    bm_d = din("b_merge", [1, 2048])
    wua_d = din("w_up_a", [512, DM])
    wub_d = din("w_up_b", [512, DM])
    wo_d = din("w_out", [DM, DM])
    n2g_d = din("norm2_g", [1, DM])
    wq_d = din("peer_wq", [DM, 2048])
    k1T_d = din("peer_k1T", [128, 128])
    k2T_d = din("peer_k2T", [128, 128])
    uT_d = din("peer_uT", [DM, 16384])
    pv_d = din("peer_v", [16384, DM])
    x2_d = nc.dram_tensor("x2_scratch", [S_LEN, DM], F32)
    out_d = nc.dram_tensor("out", [S_LEN, DM], F32, kind="ExternalOutput")

    ident = B.sb("ident", [128, 128])
    identb = B.sb("identb", [128, 128], BF16)
    B.memset("pool", ident[:], 0.0, ["ident"])
    B.asel(ident[:], ident[:], [[-1, 128]], ALU.not_equal, 1.0, 0, 1, ["ident"], ["ident"])
    B.cp("dve", identb[:], ident[:], ["ident"], ["identb"])
    causT = B.sb("causT", [128, 128], BF16)
    B.memset("pool", causT[:], 0.0, ["causT"])
    B.asel(causT[:], causT[:], [[1, 128]], ALU.is_ge, NEG, 0, -1, ["causT"], ["causT"])
    epsb = B.sb("epsb", [128, 1])
    B.memset("dve", epsb[:], EPS, ["epsb"])
    negpi = B.sb("negpi", [128, 1])
    B.memset("dve", negpi[:], -math.pi, ["negpi"])
    attn_stack = ExitStack()
    _prev_stack = B.stack
    B.stack = attn_stack
    cosT = B.sb("cosT", [128, NT, 8])
    sinT = B.sb("sinT", [128, NT, 8])
    n1g = B.sb("n1g", [128, DM])
    antiT = B.sb("antiT", [128, 128], BF16)
    B.stack = _prev_stack
    with B.scope():
        pos = B.sb("pos", [128, NT])
        S.add("pool", lambda e: e.iota(pos[:], pattern=[[128, NT]], base=0, channel_multiplier=1,
                                       allow_small_or_imprecise_dtypes=True), (), ["pos"])
        ang = B.sb("ang", [128, NT, 8])
        for i in range(8):
            inv = float(np.float32(500000.0) ** np.float32(-(2.0 * i) / 16.0))
            B.ts("dve", ang[:, :, i], pos[:], inv, None, ALU.mult, None, ["pos"], ["ang"])
        ki = B.sb("ki", [128, NT, 8], mybir.dt.int32)
        kf = B.sb("kf", [128, NT, 8])
        rr = B.sb("rr", [128, NT, 8])
        for tab, shift in ((sinT, 0.0), (cosT, 0.5 * math.pi)):
            B.ts("dve", rr[:], ang[:], shift, 1.0 / (2 * math.pi), ALU.add, ALU.mult, ["ang"], ["rr"])
            B.cp("dve", ki[:], rr[:], ["rr"], ["ki"])
            B.cp("dve", kf[:], ki[:], ["ki"], ["kf"])
            B.ts("dve", kf[:], kf[:], -2 * math.pi, None, ALU.mult, None, ["kf"], ["kf"])
            B.stt("dve", rr[:], ang[:], shift, kf[:], ALU.add, ALU.add, ["ang", "kf"], ["rr"])
            B.ts("dve", kf[:], rr[:], math.pi, -2 * math.pi, ALU.is_gt, ALU.mult, ["rr"], ["kf"])
            B.tt("dve", rr[:], rr[:], kf[:], ALU.add, ["rr", "kf"], ["rr"])
            B.ts("dve", kf[:], rr[:], -math.pi, 2 * math.pi, ALU.is_lt, ALU.mult, ["rr"], ["kf"])
            B.tt("dve", rr[:], rr[:], kf[:], ALU.add, ["rr", "kf"], ["rr"])
            B.act(tab[:], rr[:], AF.Sin, ["rr"], ["cosT" if shift > 0 else "sinT", "tab%d" % (shift > 0)])
    B.dma("sp", n1g[:], bcast_rows(n1g_d, 128, DM), [], ["n1g"])

    B.dump("cosT", cosT[:].rearrange("p t i -> p (t i)"), "tab1", [128, NT * 8])
    B.dump("sinT", sinT[:].rearrange("p t i -> p (t i)"), "tab0", [128, NT * 8])
    B.dump("n1g", n1g[:], "n1g", [128, DM])
    stop("C")

    def rope_ap(tab, t, nh):
        return bass.AP(tab, t * 8, [[NT * 8, 128], [0, nh], [1, 8]])

    class Front:
        def __init__(self):
            self.xt = [B.sb("xt", [128, DM]) for _ in range(2)]
            self.ss = B.sb("ss", [128, 1])
            self.rs = B.sb("rs", [128, 1])
            self.hb = B.sb("hb", [128, DM], BF16)
            self.hT = [B.sb("hT", [128, 8, 128], BF16) for _ in range(2)]
            self.pT = B.ps("pT", [128, 8, 128], BF16)

        def run(self, t, src_d, gain_bc, gtok):
            xt = self.xt[t % 2]
            xk = ("xt", t % 2)
            B.dma("sp", xt[:], src_d[t * 128:(t + 1) * 128, :], [], [xk])
            B.act(self.hb[:], xt[:], AF.Square, [xk], ["hb", "ss"], accum=self.ss[:])
            B.act(self.rs[:], self.ss[:], AF.Sqrt, ["ss", "epsb"], ["rs"], bias=epsb[:], scale=1.0 / DM)
            B.recip(self.rs[:], self.rs[:], ["rs"], ["rs"])
            B.stt("dve", self.hb[:], xt[:], self.rs[:], gain_bc, ALU.mult, ALU.mult, [xk, "rs", gtok], ["hb"])
            for k in range(8):
                B.tr(self.pT[:, k, :], self.hb[:, k * 128:(k + 1) * 128], identb[:], ["hb", "identb"], ["pT"])
            hT = self.hT[t % 2]
            hk = ("hT", t % 2)
            B.cp("act", hT[:], self.pT[:], ["pT"], [hk])
            return xt, xk, hT, hk

    def proj(ps_ap, hT, hk, W, wk, c0, c1, pk):
        for k in range(8):
            B.mm(ps_ap, hT[:, k, :], W[:, k, c0:c1], k == 0, k == 7, [hk, wk], [pk])

    class QKNorm:
        def __init__(self, nh):
            self.nh = nh
            self.qf = B.sb("qf", [128, nh, 64])
            self.sq = B.sb("sq", [128, nh, 64])
            self.ssq = B.sb("ssq", [128, nh])
            self.qn = B.sb("qn", [128, nh, 64])
            self.tmp = B.sb("rt", [128, 4, nh, 8])

        def run(self, ps_ap, pk, t, gain_ap, gtok):
            nh = self.nh
            qf, sq, ssq, qn, tmp = self.qf, self.sq, self.ssq, self.qn, self.tmp
            B.cp("act", qf[:].rearrange("p h d -> p (h d)"), ps_ap, [pk], ["qf"])
            B.tt("dve", sq[:], qf[:], qf[:], ALU.mult, ["qf"], ["sq"])
            B.red(ssq[:], sq[:], ALU.add, ["sq"], ["ssq"])
            B.act(ssq[:], ssq[:], AF.Sqrt, ["ssq", "epsb"], ["ssq"], bias=epsb[:], scale=1.0 / 64)
            B.recip(ssq[:], ssq[:], ["ssq"], ["ssq"])
            rsb = bass.AP(ssq, 0, [[nh, 128], [1, nh], [0, 64]])
            B.tt("dve", qn[:], qf[:], rsb, ALU.mult, ["qf", "ssq"], ["qn"])
            B.tt("dve", qn[:], qn[:], gain_ap, ALU.mult, ["qn"] + (gtok if isinstance(gtok, list) else [gtok]), ["qn"])
            c = rope_ap(cosT, t, nh)
            s = rope_ap(sinT, t, nh)
            x1 = qn[:, :, 0:8]
            x2 = qn[:, :, 8:16]
            B.tt("dve", tmp[:, 0], x1, c, ALU.mult, ["qn", "cosT"], ["rt0"])
            B.tt("dve", tmp[:, 1], x2, s, ALU.mult, ["qn", "sinT"], ["rt1"])
            B.tt("dve", tmp[:, 2], x2, c, ALU.mult, ["qn", "cosT"], ["rt2"])
            B.tt("dve", tmp[:, 3], x1, s, ALU.mult, ["qn", "sinT"], ["rt3"])
            B.tt("dve", x1, tmp[:, 0], tmp[:, 1], ALU.subtract, ["rt0", "rt1"], ["qn"])
            B.tt("dve", x2, tmp[:, 2], tmp[:, 3], ALU.add, ["rt2", "rt3"], ["qn"])
            return qn

    def gain_tile(name, d, reps):
        g = B.sb(name, [128, 64])
        B.dma("sp", g[:], bcast_rows(d, 128, 64), [], [name])
        return g

    B.memset("pool", antiT[:], 0.0, ["antiT"])
    B.asel(antiT[:], antiT[:], [[-1, 128]], ALU.is_gt, NEG, 0, 1, ["antiT"], ["antiT"])
    B.stack = attn_stack
    ya = B.sb("ya", [128, NT, 512], BF16)
    B.stack = _prev_stack
    if "moba" in SKIP:
        B.memset("pool", ya[:], 0.0, [("ya", t) for t in range(NT)])
    else:
      with B.scope():
        QaT = B.sb("QaT", [128, 4, S_LEN], BF16)
        KaT = B.sb("KaT", [128, 4, S_LEN], BF16)
        Va = B.sb("Va", [128, NT, 8, 65], BF16)
        MBs = B.sb("MBs", [128, NT, 8, 16], BF16)
        kmT = B.sb("kmT", [128, 4, 16])
        B.memset("pool", kmT[:], 0.0, ["kmT"])
        B.memset("pool", Va[:, :, :, 64:65], 1.0, ["Va1"])
        IndA = B.sb("IndA", [128, 16, 128], BF16)
        B.memset("pool", IndA[:], 0.0, ["IndA"])
        B.asel(IndA[0:16], IndA[0:16], [[1, 16], [0, 128]], ALU.not_equal, 1.0, 0, -1, ["IndA"], ["IndA"])
        with B.scope():
            Wa = B.sb("Wa", [128, 8, 1536], BF16)
            for k in range(8):
                B.dma("pool", Wa[:, k, :], wa_d[k * 128:(k + 1) * 128, :], [], [("Wa", k)])
            aqg = gain_tile("aqg", aqg_d, 8)
            akg = gain_tile("akg", akg_d, 8)
            aqg_ap = bass.AP(aqg, 0, [[64, 128], [0, 8], [1, 64]])
            akg_ap = bass.AP(akg, 0, [[64, 128], [0, 8], [1, 64]])
            fr = Front()
            qkn = QKNorm(8)
            pp = [B.ps("pp", [128, 512], tok="pp%d" % i) for i in range(2)]
            ptr = B.ps("ptr", [128, 4, 128])
            pgb = [B.ps("pg", [128, 512], tok="pg%d" % i) for i in range(2)]
            pg = [pgb[i][:, 0:64].rearrange("p (h n) -> p h n", n=16) for i in range(2)]
            qTf = B.sb("qTf", [128, 4, 128])
            ksum = B.sb("ksum", [128, 4])
            gpad = B.sb("gpad", [128, 8, 16])
            m8 = B.sb("m8", [128, 8, 8])
            for t in range(NT_RUN):
                xt, xk, hT, hk = fr.run(t, x_d, n1g[:], "n1g")
                tsl = slice(t * 128, (t + 1) * 128)
                p = pp[0]
                for k in range(8):
                    B.mm(p[:], hT[:, k, :], Wa[:, k, 512:1024], k == 0, k == 7, [hk, ("Wa", k)], ["pp0"])
                kn = qkn.run(p[:], "pp0", t, akg_ap, "akg")
                for pr in range(4):
                    B.tr(ptr[:, pr, :], kn[:, 2 * pr:2 * pr + 2, :].rearrange("p h d -> p (h d)"), ident[:],
                         ["qn", "ident"], ["ptr"])
                B.cp("act", KaT[:, :, tsl], ptr[:], ["ptr"], [("KaT", t)])
                B.cp("dve", qTf[:], ptr[:], ["ptr"], ["qTf"])
                B.red(ksum[:], qTf[:], ALU.add, ["qTf"], ["ksum"])
                n = t // 2
                if t % 2 == 0:
                    B.cp("dve", kmT[:, :, n], ksum[:], ["ksum"], ["kmT"])
                else:
                    B.tt("dve", kmT[:, :, n], kmT[:, :, n], ksum[:], ALU.add, ["ksum", "kmT"], ["kmT"])
                p = pp[1]
                for k in range(8):
                    B.mm(p[:], hT[:, k, :], Wa[:, k, 0:512], k == 0, k == 7, [hk, ("Wa", k)], ["pp1"])
                qn = qkn.run(p[:], "pp1", t, aqg_ap, "aqg")
                for pr in range(4):
                    B.tr(ptr[:, pr, :], qn[:, 2 * pr:2 * pr + 2, :].rearrange("p h d -> p (h d)"), ident[:],
                         ["qn", "ident"], ["ptr"])
                B.cp("act", QaT[:, :, tsl], ptr[:], ["ptr"], [("QaT", t)])
                B.cp("dve", qTf[:], ptr[:], ["ptr"], ["qTf"])
                for h in range(8):
                    po = (h % 2) * 64
                    B.mm(pg[h % 2][:, h // 2, :], qTf[po:po + 64, h // 2, :], kmT[po:po + 64, h // 2, :], True, True,
                         ["qTf", "kmT"], ["pg%d" % (h % 2)])
                own = t // 2
                if own > 0:
                    gp4 = gpad[:].rearrange("p (a b) n -> p a b n", b=2)
                    B.cp("dve", gp4[:, :, 0, :], pg[0], ["pg0"], ["gpad"])
                    B.cp("dve", gp4[:, :, 1, :], pg[1], ["pg1"], ["gpad"])
                    B.memset("dve", gpad[:, :, own:16], -BIGF, ["gpad"])
                    for h in range(8):
                        B.max8(m8[:, h, :], gpad[:, h, :], ["gpad"], ["m8"])
                    thr = bass.AP(m8, 2, [[64, 128], [8, 8], [0, 16]])
                    B.tt("dve", gpad[:], gpad[:], thr, ALU.is_lt, ["gpad", "m8"], ["gpad"])
                    B.ts("dve", MBs[:, t], gpad[:], NEG, None, ALU.mult, None, ["gpad"], [("MBs", t)])
                p = pp[0]
                for k in range(8):
                    B.mm(p[:], hT[:, k, :], Wa[:, k, 1024:1536], k == 0, k == 7, [hk, ("Wa", k)], ["pp0"])
                B.cp("act", Va[:, t, :, 0:64], p[:].rearrange("p (h d) -> p h d", d=64), ["pp0"], [("Va", t)])
            B.dump("QaT", QaT[:, 0, 0:NT_RUN * 128], [("QaT", t) for t in range(NT_RUN)], [128, NT_RUN * 128], BF16)
            B.dump("KaT", KaT[:, 3, 0:NT_RUN * 128], [("KaT", t) for t in range(NT_RUN)], [128, NT_RUN * 128], BF16)
            B.dump("MBs", MBs[:, 2:NT_RUN].rearrange("p t h n -> p (t h n)"), [("MBs", t) for t in range(2, NT_RUN)], [128, (NT_RUN - 2) * 128], BF16)

        if stop_after == "A1":
            pass
        else:
            with B.scope():
                NS = 3
                sps = [B.ps("sps", [128, 4, 128], tok=("sps", i)) for i in range(NS)]
                ops_ = [B.ps("ops", [128, 512], tok=("ops", i)) for i in range(2)]
                pmb = B.ps("pmb", [16, 8, 128], BF16)
                PT = [B.sb("PT", [128, 4, 128], BF16) for _ in range(NS)]
                MBT = [B.sb("MBT", [128, 8, 128], BF16) for _ in range(2)]
                for i_ in range(2):
                    B.memset("pool", MBT[i_][:], 0.0, [("MBT", i_)])
                rinv = [B.sb("rinv", [128, 1]) for _ in range(2)]
                gi = 0
                oi = 0
                for qi in range(NT_RUN):
                    qsl = slice(qi * 128, (qi + 1) * 128)
                    own = qi // 2
                    mk = ("MBT", qi % 2)
                    if own > 0:
                        for h in range(8):
                            B.tr(pmb[:, h, :], MBs[:, qi, h, :], identb[:], [("MBs", qi), "identb"], ["pmb"])
                        B.cp("act", MBT[qi % 2][0:16], pmb[:], ["pmb"], [mk])
                    for h in range(8):
                        po = (h % 2) * 64
                        pr = h // 2
                        qT = QaT[po:po + 64, pr, qsl]
                        o = ops_[oi % 2]
                        ok = ("ops", oi % 2)
                        oi += 1
                        kts = list(range(qi + 1))
                        ngrp = (len(kts) + 3) // 4

                        def issue_qk(g):
                            nonlocal gi
                            grp = kts[4 * g:4 * g + 4]
                            sp = sps[gi % NS]
                            sk = ("sps", gi % NS)
                            pt = PT[gi % NS]
                            pk = ("PT", gi % NS)
                            gi += 1
                            for j, kt in enumerate(grp):
                                ksl = slice(kt * 128, (kt + 1) * 128)
                                past = (kt // 2) < own
                                diag = kt == qi
                                B.mm(sp[:, j, :], KaT[po:po + 64, pr, ksl], qT, True, not (past or diag),
                                     [("KaT", kt), ("QaT", qi)], [sk])
                                if past:
                                    B.mm(sp[:, j, :], IndA[:, kt // 2, :], MBT[qi % 2][:, h, :], False, True, ["IndA", mk], [sk])
                                if diag:
                                    B.mm(sp[:, j, :], identb[:], causT[:], False, True, ["identb", "causT"], [sk])
                            return grp, sp, sk, pt, pk

                        cur = issue_qk(0)
                        for g in range(ngrp):
                            nxt = issue_qk(g + 1) if g + 1 < ngrp else None
                            grp, sp, sk, pt, pk = cur
                            nj = len(grp)
                            B.act(pt[:, 0:nj, :], sp[:, 0:nj, :], AF.Exp, [sk], [pk], scale=SCALE)
                            for j, kt in enumerate(grp):
                                first = (g == 0 and j == 0)
                                last = (g == ngrp - 1 and j == nj - 1)
                                B.mm(o[:, 0:65], pt[:, j, :], Va[:, kt, h, :], first, last, [pk, ("Va", kt), "Va1"], [ok])
                            cur = nxt
                        ri = rinv[oi % 2]
                        rk = ("rinv", oi % 2)
                        B.recip(ri[:], o[:, 64:65], [ok], [rk])
                        B.ts("dve", ya[:, qi, h * 64:(h + 1) * 64], o[:, 0:64], ri[:], None, ALU.mult, None, [ok, rk], [("ya", qi)])
            B.dump("ya", ya[:, 0:NT_RUN].rearrange("p t c -> p (t c)"), [("ya", t) for t in range(NT_RUN)], [128, NT_RUN * 512], BF16)


    stop("B1")
    B.stack = attn_stack
    yb = B.sb("yb", [128, NT, 512], BF16)
    B.stack = _prev_stack
    with B.scope():
        QbT = B.sb("QbT", [128, 4, S_LEN], BF16)
        KsT = B.sb("KsT", [128, S_LEN], BF16)
        KwT = B.sb("KwT", [128, S_LEN], BF16)
        Vs = B.sb("Vs", [128, NT, 2, 65], BF16)
        Vw = B.sb("Vw", [128, NT, 2, 65], BF16)
        KcT = B.sb("KcT", [128, 256], BF16)
        Vc = B.sb("Vc", [128, 2, 2, 129], BF16)
        gbt = B.sb("gbt", [128, NT, 24])
        B.memset("pool", Vs[:, :, :, 64:65], 1.0, ["Vs1"])
        B.memset("pool", Vw[:, :, :, 64:65], 1.0, ["Vw1"])
        with B.scope():
            bkcT = B.sb("bkcT", [128, 16, 256], BF16)
            bvcT = B.sb("bvcT", [128, 16, 256], BF16)
            with B.scope():
                Wb = B.sb("Wb", [128, 8, 1304], BF16)
                for k in range(8):
                    B.dma("pool", Wb[:, k, :], wb_d[k * 128:(k + 1) * 128, :], [], [("Wb", k)])
                bqg = gain_tile("bqg", bqg_d, 8)
                bqg_ap = bass.AP(bqg, 0, [[64, 128], [0, 8], [1, 64]])
                g4 = B.sb("g4", [128, 4, 64])
                B.dma("sp", g4[:, 0, :], bcast_rows(bksg_d, 128, 64), [], ["g4a"])
                B.dma("sp", g4[:, 1, :], bcast_rows(bksg_d, 128, 64), [], ["g4b"])
                B.dma("sp", g4[:, 2, :], bcast_rows(bkwg_d, 128, 64), [], ["g4c"])
                B.dma("sp", g4[:, 3, :], bcast_rows(bkwg_d, 128, 64), [], ["g4d"])
                g4toks = ["g4a", "g4b", "g4c", "g4d"]
                fr = Front()
                qkn8 = QKNorm(8)
                qkn4 = QKNorm(4)
                pp = [B.ps("pp", [128, 512], tok="pp%d" % i) for i in range(2)]
                ptr = B.ps("ptr", [128, 4, 128])
                raw = B.sb("raw", [128, 256])
                for t in range(NT_RUN):
                    xt, xk, hT, hk = fr.run(t, x_d, n1g[:], "n1g")
                    tsl = slice(t * 128, (t + 1) * 128)
                    p = pp[0]
                    for k in range(8):
                        B.mm(p[:], hT[:, k, :], Wb[:, k, 0:512], k == 0, k == 7, [hk, ("Wb", k)], ["pp0"])
                    qn = qkn8.run(p[:], "pp0", t, bqg_ap, "bqg")
                    for r in range(4):
                        B.tr(ptr[:, r, :], qn[:, 2 * r:2 * r + 2, :].rearrange("p h d -> p (h d)"), ident[:],
                             ["qn", "ident"], ["ptr"])
                    B.cp("act", QbT[:, :, tsl], ptr[:], ["ptr"], [("QbT", t)])
                    p = pp[1]
                    for k in range(8):
                        B.mm(p[:], hT[:, k, :], Wb[:, k, 512:1024], k == 0, k == 7, [hk, ("Wb", k)], ["pp1"])
                    B.cp("act", raw[:], p[:, 256:512], ["pp1"], ["raw"])
                    kn = qkn4.run(p[:, 0:256], "pp1", t, g4[:], g4toks)
                    B.tr(ptr[:, 0, :], kn[:, 0:2, :].rearrange("p h d -> p (h d)"), ident[:], ["qn", "ident"], ["ptr"])
                    B.tr(ptr[:, 1, :], kn[:, 2:4, :].rearrange("p h d -> p (h d)"), ident[:], ["qn", "ident"], ["ptr"])
                    B.tr(ptr[:, 2, :], raw[:, 0:128], ident[:], ["raw", "ident"], ["ptr"])
                    B.tr(ptr[:, 3, :], raw[:, 128:256], ident[:], ["raw", "ident"], ["ptr"])
                    B.cp("act", KsT[:, tsl], ptr[:, 0, :], ["ptr"], [("KsT", t)])
                    B.cp("act", KwT[:, tsl], ptr[:, 1, :], ["ptr"], [("KwT", t)])
                    B.cp("act", bkcT[:, :, 8 * t:8 * t + 8], ptr[:, 2, :].rearrange("p (n f) -> p f n", f=16), ["ptr"], [("bkcT", t)])
                    B.cp("act", bvcT[:, :, 8 * t:8 * t + 8], ptr[:, 3, :].rearrange("p (n f) -> p f n", f=16), ["ptr"], [("bvcT", t)])
                    p = pp[0]
                    for k in range(8):
                        B.mm(p[:, 0:280], hT[:, k, :], Wb[:, k, 1024:1304], k == 0, k == 7, [hk, ("Wb", k)], ["pp0"])
                    B.cp("act", Vs[:, t, :, 0:64], p[:, 0:128].rearrange("p (g d) -> p g d", d=64), ["pp0"], [("Vs", t)])
                    B.cp("act", Vw[:, t, :, 0:64], p[:, 128:256].rearrange("p (g d) -> p g d", d=64), ["pp0"], [("Vw", t)])
                    B.act(gbt[:, t, :], p[:, 256:280], AF.Sigmoid, ["pp0"], [("gbt", t)])
            stop("A2")
            with B.scope():
                ovl = B.sb("ovl", [128, 2, 64])
                B.memset("pool", ovl[:], 1.0, ["ovl"])
                for nt_ in range(2):
                    B.asel(ovl[:, nt_, :], ovl[:, nt_, :], [[-4, 64]], ALU.is_ge, 0.0, 128 * nt_ + 1, 1, ["ovl"], ["ovl"])
                    B.asel(ovl[:, nt_, :], ovl[:, nt_, :], [[4, 64]], ALU.is_ge, 0.0, 3 - 128 * nt_, -1, ["ovl"], ["ovl"])
                for nt_ in range(2):
                    for g in range(2):
                        B.cp("dve", Vc[:, nt_, g, 65:129], ovl[:, nt_, :], ["ovl"], [("Vco", nt_, g)])
                B.memset("pool", Vc[:, :, :, 64:65], 1.0, ["Vc1"])
                bkcg = gain_tile("bkcg", bkcg_d, 1)
                ph = [B.ps("ph", [128, 512], tok="ph%d" % i) for i in range(2)]
                po2 = B.ps("po2", [128, 512])
                ptr2 = B.ps("ptr2", [128, 4, 128])
                kcn = B.sb("kcn", [128, 2, 2, 64])
                for kv in ("k", "v"):
                    if kv == "v" and stop_after == "CMPe":
                        B.dump("kcn", kcn[:].rearrange("p a b c -> p (a b c)"), [("kcn", 0), ("kcn", 1)], [128, 256])
                        stop("CMPe")
                    cd = cmp_d[kv]
                    srcT = bkcT if kv == "k" else bvcT
                    stoks = [(("bkcT" if kv == "k" else "bvcT"), t) for t in range(NT_RUN)]
                    w1s = B.sb("w1s", [128, 32, 256], BF16)
                    w1v = cd["w1"].ap().rearrange("(l d) c -> d l c", d=64)
                    B.dma("pool", w1s[0:64], w1v, [], [("w1s", kv, 0)])
                    B.dma("pool", w1s[64:128], w1v, [], [("w1s", kv, 1)])
                    posT = B.sb("posT", [128, 32, 2], BF16)
                    B.dma("pool", posT[0:64].rearrange("p l c -> p (l c)"), cd["posT"].ap(), [], [("posT", kv)])
                    B.dma("pool", posT[64:128].rearrange("p l c -> p (l c)"), cd["posT"].ap(), [], [("posT2", kv)])
                    b1t = B.sb("b1t", [128, 2])
                    B.dma("sp", b1t[:], cd["b1"].ap(), [], [("b1t", kv)])
                    w2s = B.sb("w2s", [128, 2, 64], BF16)
                    B.dma("pool", w2s[:], cd["w2"].ap().rearrange("(k p) d -> p k d", p=128), [], [("w2s", kv)])
                    b2t = B.sb("b2t", [128, 64])
                    B.dma("sp", b2t[:], bcast_rows(cd["b2"], 128, 64), [], [("b2t", kv)])
                    if stop_after == "CMPa":
                        B.dump("w1s", w1s[:, 5, :], [("w1s", kv, 0), ("w1s", kv, 1)], [128, 256], BF16)
                        B.dump("w2s", w2s[:].rearrange("p a b -> p (a b)"), [("w2s", kv)], [128, 128], BF16)
                        B.dump("b1t", b1t[:], [("b1t", kv)], [128, 2])
                        B.dump("posT", posT[:].rearrange("p a b -> p (a b)"), [("posT", kv), ("posT2", kv)], [128, 64], BF16)
                        stop("CMPa")
                    hidT = B.sb("hidT", [128, 2, 256], BF16)
                    B.memset("pool", hidT[:, :, 255:256], 0.0, [("hidT", 0), ("hidT", 1)])
                    biasc = B.sb("biasc", [128, 1])
                    cf = B.sb("cf", [128, 2, 64])
                    sq2 = B.sb("sq2", [128, 2, 64])
                    ss2 = B.sb("ss2", [128, 2])
                    for g in range(2):
                        for cc in range(2):
                            phb = ph[cc]
                            pk_ = "ph%d" % cc
                            csl = slice(cc * 128, (cc + 1) * 128)
                            for l in range(32):
                                B.mm(phb[:, 256:258], w1s[g * 64:(g + 1) * 64, l, csl], posT[g * 64:(g + 1) * 64, l, :], l == 0, l == 31,
                                     [("w1s", kv, g), ("posT", kv), ("posT2", kv)], [pk_])
                            for l in range(32):
                                rhs = srcT[g * 64:(g + 1) * 64, l % 16, l // 16:l // 16 + 255]
                                B.mm(phb[:, 0:255], w1s[g * 64:(g + 1) * 64, l, csl], rhs, l == 0, l == 31,
                                     [("w1s", kv, g)] + stoks, [pk_])
                            B.tt("dve", biasc[:], phb[:, 256:257], b1t[:, cc:cc + 1], ALU.add, [pk_, ("b1t", kv)], ["biasc"])
                            if stop_after == "CMPb" or (stop_after == "CMPg1" and g == 1):
                                B.dump("biasc", biasc[:], ["biasc"], [128, 1])
                                stop(stop_after)
                            B.act(hidT[:, cc, 0:255], phb[:, 0:255], AF.Gelu_apprx_tanh, [pk_, "biasc"], [("hidT", cc)], bias=biasc[:])
                        if stop_after == "CMPc":
                            B.dump("hidT", hidT[:].rearrange("p a b -> p (a b)"), [("hidT", 0), ("hidT", 1)], [128, 512], BF16)
                            stop("CMPc")
                        for nt_ in range(2):
                            for cc in range(2):
                                B.mm(po2[:, nt_ * 64:(nt_ + 1) * 64], hidT[:, cc, nt_ * 128:(nt_ + 1) * 128], w2s[:, cc, :],
                                     cc == 0, cc == 1, [("hidT", cc), ("w2s", kv)], ["po2"])
                        b2b = bass.AP(b2t, 0, [[64, 128], [0, 2], [1, 64]])
                        B.tt("dve", cf[:], po2[:, 0:128].rearrange("p (n d) -> p n d", d=64), b2b, ALU.add, ["po2", ("b2t", kv)], ["cf"])
                        if stop_after == "CMPd":
                            B.dump("cf", cf[:].rearrange("p a b -> p (a b)"), ["cf"], [128, 128])
                            stop("CMPd")
                        if kv == "v":
                            B.cp("act", Vc[:, :, g, 0:64], cf[:], ["cf"], [("Vcv", g)])
                        else:
                            B.tt("dve", sq2[:], cf[:], cf[:], ALU.mult, ["cf"], ["sq2"])
                            B.red(ss2[:], sq2[:], ALU.add, ["sq2"], ["ss2"])
                            B.act(ss2[:], ss2[:], AF.Sqrt, ["ss2", "epsb"], ["ss2"], bias=epsb[:], scale=1.0 / 64)
                            B.recip(ss2[:], ss2[:], ["ss2"], ["ss2"])
                            B.tt("dve", cf[:], cf[:], bass.AP(ss2, 0, [[2, 128], [1, 2], [0, 64]]), ALU.mult, ["cf", "ss2"], ["cf"])
                            B.tt("dve", kcn[:, :, g, :], cf[:], bass.AP(bkcg, 0, [[64, 128], [0, 2], [1, 64]]), ALU.mult,
                                 ["cf", "bkcg"], [("kcn", g)])
                            if stop_after == "CMPd2":
                                B.dump("kcn", kcn[:, :, 0, :], [("kcn", 0)], [128, 2, 64])
                                stop("CMPd2")
                if stop_after == "CMPf":
                    B.dump("kcn", kcn[:].rearrange("p a b c -> p (a b c)"), [("kcn", 0), ("kcn", 1)], [128, 256])
                    stop("CMPf")
                for nt_ in range(2):
                    B.tr(ptr2[:, nt_, :], kcn[:, nt_, :, :].rearrange("p g d -> p (g d)"), ident[:],
                         [("kcn", 0), ("kcn", 1), "ident"], ["ptr2"])
                B.cp("act", KcT[:].rearrange("p (n a) -> p n a", a=128), ptr2[:, 0:2, :], ["ptr2"], ["KcT"])
                B.dump("KcT", KcT[:], "KcT", [128, 256], BF16)
                B.dump("Vc", Vc[:].rearrange("p a b c -> p (a b c)"), [("Vcv", 0), ("Vcv", 1), "Vc1", ("Vco", 0, 0), ("Vco", 0, 1), ("Vco", 1, 0), ("Vco", 1, 1)], [128, 516], BF16)
        B.dump("QbT", QbT[:, 1, 0:NT_RUN * 128], [("QbT", t) for t in range(NT_RUN)], [128, NT_RUN * 128], BF16)
        B.dump("KsT", KsT[:, 0:NT_RUN * 128], [("KsT", t) for t in range(NT_RUN)], [128, NT_RUN * 128], BF16)
        B.dump("KwT", KwT[:, 0:NT_RUN * 128], [("KwT", t) for t in range(NT_RUN)], [128, NT_RUN * 128], BF16)
        B.dump("gbt", gbt[:, 0:NT_RUN].rearrange("p t c -> p (t c)"), [("gbt", t) for t in range(NT_RUN)], [128, NT_RUN * 24])
        stop("CMP")
        with B.scope():
            NS = 3
            sps = [B.ps("sps", [128, 4, 128], tok=("sps", i)) for i in range(NS)]
            ops_ = [B.ps("ops", [128, 512], tok=("ops", i)) for i in range(2)]
            pmb = B.ps("pmb", [64, 8, 128], BF16)
            PT = [B.sb("PT", [128, 4, 128], BF16) for _ in range(NS)]
            IndS = B.sb("IndS", [128, NT, 128], BF16)
            B.memset("pool", IndS[:], 0.0, ["IndS"])
            for hf in range(2):
                B.asel(IndS[0:64, :, hf * 64:(hf + 1) * 64], IndS[0:64, :, hf * 64:(hf + 1) * 64], [[2, NT], [0, 64]],
                       ALU.not_equal, 1.0, hf, -1, ["IndS"], ["IndS"])
            Ttab = B.sb("Ttab", [128, 128])
            B.memset("pool", Ttab[:], 0.0, ["Ttab"])
            for hf in range(2):
                rows = slice(64 * hf, 64 * hf + 64)
                B.asel(Ttab[rows, :], Ttab[rows, :], [[-1, 128]], ALU.is_ge, -BIGF, 64 + hf, 0, ["Ttab"], ["Ttab"])
                B.asel(Ttab[rows, :], Ttab[rows, :], [[1, 128]], ALU.not_equal, BIGF, -(64 + hf), 0, ["Ttab"], ["Ttab"])
                B.asel(Ttab[rows, :], Ttab[rows, :], [[1, 128]], ALU.not_equal, BIGF, -(63 + hf), 0, ["Ttab"], ["Ttab"])
            tiny = B.sb("tiny", [128, 1])
            B.memset("dve", tiny[:], 1e-30, ["tiny"])
            cmk = [B.sb("cmk", [128, 128], BF16) for _ in range(4)]
            ybf = B.sb("ybf", [128, 512])
            impg = [B.sb("impg", [128, 64]) for _ in range(2)]
            imp2 = B.sb("imp2", [128, 64])
            m8a = B.sb("m8a", [128, 8])
            m8b = B.sb("m8b", [128, 8])
            MBg = B.sb("MBg", [128, 64], BF16)
            MBTs = [[B.sb("MBTs", [128, 128], BF16) for _ in range(2)] for _ in range(2)]
            for g_ in range(2):
                for i_ in range(2):
                    B.memset("pool", MBTs[g_][i_][:], 0.0, [("MBTs", g_, i_)])
            rv = [B.sb("rv", [128, 1]) for _ in range(4)]
            cf_ = [B.sb("cfc", [128, 1]) for _ in range(4)]

            class St:
                gi = 0
                oi = 0
                ci = 0
                ri = 0

            def attend(qT, qtok, tiles, ncols):
                o = ops_[St.oi % 2]
                ok = ("ops", St.oi % 2)
                St.oi += 1
                ngrp = (len(tiles) + 3) // 4

                def issue_qk(g_):
                    grp = tiles[4 * g_:4 * g_ + 4]
                    sp = sps[St.gi % NS]
                    sk = ("sps", St.gi % NS)
                    pt = PT[St.gi % NS]
                    pk = ("PT", St.gi % NS)
                    St.gi += 1
                    for j, (kT, ktok, V, vtoks, masks) in enumerate(grp):
                        B.mm(sp[:, j, :], kT, qT, True, len(masks) == 0, [ktok, qtok], [sk])
                        for mi, (ml, mr, mt) in enumerate(masks):
                            B.mm(sp[:, j, :], ml, mr, False, mi == len(masks) - 1, mt, [sk])
                    return grp, sp, sk, pt, pk

                cur = issue_qk(0)
                for g_ in range(ngrp):
                    nxt = issue_qk(g_ + 1) if g_ + 1 < ngrp else None
                    grp, sp, sk, pt, pk = cur
                    nj = len(grp)
                    B.act(pt[:, 0:nj, :], sp[:, 0:nj, :], AF.Exp, [sk], [pk], scale=SCALE)
                    for j, (kT, ktok, V, vtoks, masks) in enumerate(grp):
                        B.mm(o[:, 0:ncols], pt[:, j, :], V, g_ == 0 and j == 0, g_ == ngrp - 1 and j == nj - 1,
                             [pk] + vtoks, [ok])
                    cur = nxt
                return o, ok

            def coef(o, ok, gcol_ap, gtok):
                r_ = rv[St.ri % 4]
                rk = ("rv", St.ri % 4)
                c_ = cf_[St.ri % 4]
                ck = ("cfc", St.ri % 4)
                St.ri += 1
                B.tt("dve", r_[:], o[:, 64:65], tiny[:], ALU.max, [ok, "tiny"], [rk])
                B.recip(r_[:], r_[:], [rk], [rk])
                B.tt("dve", c_[:], r_[:], gcol_ap, ALU.mult, [rk, gtok], [ck])
                return r_, rk, c_, ck

            qlist = list(range(NT_RUN)) if QT_LIST is None else list(QT_LIST)
            for qi in qlist:
                qsl = slice(qi * 128, (qi + 1) * 128)
                qtok = ("QbT", qi)
                ctiles = []
                for kt in range(2):
                    base = 128 * qi - 2048 * kt - 31
                    if base + 127 < 0:
                        continue
                    masks = []
                    if base - 16 * 127 < 0:
                        cm_ = cmk[St.ci % 4]
                        ck_ = ("cmk", St.ci % 4)
                        St.ci += 1
                        B.memset("pool", cm_[:], 0.0, [ck_])
                        B.asel(cm_[:], cm_[:], [[1, 128]], ALU.is_ge, NEG, base, -16, [ck_], [ck_])
                        masks = [(identb[:], cm_[:], ["identb", ck_])]
                    ctiles.append((kt, masks))
                for g in range(2):
                    gs = slice(g * 64, (g + 1) * 64)
                    for r in range(4):
                        h = g * 4 + r
                        hs = slice(h * 64, (h + 1) * 64)
                        qT = QbT[gs, r, qsl]
                        if ctiles:
                            tiles = [(KcT[gs, kt * 128:(kt + 1) * 128], "KcT", Vc[:, kt, g, :],
                                      [("Vcv", g), "Vc1", ("Vco", kt, g)], masks) for kt, masks in ctiles]
                            o, ok = attend(qT, qtok, tiles, 129)
                            r_, rk, c_, ck = coef(o, ok, gbt[:, qi, h * 3:h * 3 + 1], ("gbt", qi))
                            B.ts("dve", ybf[:, hs], o[:, 0:64], c_[:], None, ALU.mult, None, [ok, ck], [("ybf", h)])
                            if r == 0:
                                B.ts("dve", impg[g][:], o[:, 65:129], r_[:], None, ALU.mult, None, [ok, rk], [("impg", g)])
                            else:
                                B.stt("dve", impg[g][:], o[:, 65:129], r_[:], impg[g][:], ALU.mult, ALU.add,
                                      [ok, rk, ("impg", g)], [("impg", g)])
                        else:
                            B.memset("dve", ybf[:, hs], 0.0, [("ybf", h)])
                            if r == 0:
                                B.memset("dve", impg[g][:], 0.0, [("impg", g)])
                    ik = ("impg", g)
                    B.tt("dve", impg[g][:], impg[g][:], Ttab[:, 64 - 2 * qi:128 - 2 * qi], ALU.add, [ik, "Ttab"], [ik])
                    B.memset("dve", impg[g][:, 0:1], BIGF, [ik])
                    B.max8(m8a[:], impg[g][:], [ik], ["m8a"])
                    B.mrep(imp2[:], m8a[:], impg[g][:], -3.0e38, [ik, "m8a"], ["imp2"])
                    B.max8(m8b[:], imp2[:], ["imp2"], ["m8b"])
                    B.ts("dve", MBg[:], impg[g][:], m8b[:, 7:8], NEG, ALU.is_lt, ALU.mult, [ik, "m8b"], ["MBg"])
                    B.tr(pmb[:, 0, :], MBg[:], identb[:], ["MBg", "identb"], ["pmb"])
                    mbt = MBTs[g][qi % 2]
                    mk = ("MBTs", g, qi % 2)
                    B.cp("act", mbt[0:64], pmb[:, 0, :], ["pmb"], [mk])
                    if qi == qlist[-1] and g == 0:
                        B.dump("MBg", MBg[:], "MBg", [128, 64], BF16)
                    for r in range(4):
                        h = g * 4 + r
                        hs = slice(h * 64, (h + 1) * 64)
                        qT = QbT[gs, r, qsl]
                        tiles = []
                        for kt in range(qi + 1):
                            ksl = slice(kt * 128, (kt + 1) * 128)
                            if kt == qi:
                                masks = [(identb[:], causT[:], ["identb", "causT"])]
                            else:
                                masks = [(IndS[:, kt, :], mbt[:], ["IndS", mk])]
                            tiles.append((KsT[gs, ksl], ("KsT", kt), Vs[:, kt, g, :], [("Vs", kt), "Vs1"], masks))
                        o, ok = attend(qT, qtok, tiles, 65)
                        r_, rk, c_, ck = coef(o, ok, gbt[:, qi, h * 3 + 1:h * 3 + 2], ("gbt", qi))
                        B.stt("dve", ybf[:, hs], o[:, 0:64], c_[:], ybf[:, hs], ALU.mult, ALU.add, [ok, ck, ("ybf", h)], [("ybf", h)])
                        tiles = []
                        for kt in range(max(0, qi - 4), qi + 1):
                            ksl = slice(kt * 128, (kt + 1) * 128)
                            masks = []
                            if kt == qi:
                                masks = [(identb[:], causT[:], ["identb", "causT"])]
                            elif kt == qi - 4:
                                masks = [(identb[:], antiT[:], ["identb", "antiT"])]
                            tiles.append((KwT[gs, ksl], ("KwT", kt), Vw[:, kt, g, :], [("Vw", kt), "Vw1"], masks))
                        o, ok = attend(qT, qtok, tiles, 65)
                        r_, rk, c_, ck = coef(o, ok, gbt[:, qi, h * 3 + 2:h * 3 + 3], ("gbt", qi))
                        B.stt("dve", ybf[:, hs], o[:, 0:64], c_[:], ybf[:, hs], ALU.mult, ALU.add, [ok, ck, ("ybf", h)], [("ybf", h)])
                B.cp("act", yb[:, qi, :], ybf[:], [("ybf", h) for h in range(8)], [("yb", qi)])
    if QT_LIST is None:
        B.dump("yb", yb[:, 0:NT_RUN].rearrange("p t c -> p (t c)"), [("yb", t) for t in range(NT_RUN)], [128, NT_RUN * 512], BF16)
    else:
        for qi in QT_LIST:
            B.dump("yb%d" % qi, yb[:, qi, :], [("yb", qi)], [128, 512], BF16)
    stop("B2")


    with B.scope():
        Wm = B.sb("Wm", [128, 8, 2048], BF16)
        for k in range(8):
            B.dma("pool", Wm[:, k, :], wm_d[k * 128:(k + 1) * 128, :], [], [("Wm", k)])
        wua = B.sb("wua", [128, 4, DM], BF16)
        wub = B.sb("wub", [128, 4, DM], BF16)
        for k in range(4):
            B.dma("pool", wua[:, k, :], wua_d[k * 128:(k + 1) * 128, :], [], [("wua", k)])
            B.dma("pool", wub[:, k, :], wub_d[k * 128:(k + 1) * 128, :], [], [("wub", k)])
        wo = B.sb("wo", [128, 8, DM], BF16)
        for k in range(8):
            B.dma("pool", wo[:, k, :], wo_d[k * 128:(k + 1) * 128, :], [], [("wo", k)])
        bmb = B.sb("bmb", [128, 2048])
        B.dma("sp", bmb[:], bcast_rows(bm_d, 128, 2048), [], ["bmb"])
        fr = Front()
        pp = [B.ps("pp", [128, 512], tok="pp%d" % i) for i in range(2)]
        pa = [B.ps("pa", [128, 512], tok="pa%d" % i) for i in range(2)]
        gt = B.sb("gt", [128, 2048], BF16)
        tmpg = B.sb("tmpg", [128, 512])
        yT = B.sb("yT", [128, 8, 128], BF16)
        m1 = B.sb("m1", [128, DM])
        mg = B.sb("mg", [128, DM], BF16)
        mT = B.sb("mT", [128, 8, 128], BF16)
        x2t = [B.sb("x2t", [128, DM]) for _ in range(2)]
        for t in range(NT_RUN):
            xt, xk, hT, hk = fr.run(t, x_d, n1g[:], "n1g")
            for cg in range(4):
                p = pp[cg % 2]
                pk = "pp%d" % (cg % 2)
                cs = slice(cg * 512, (cg + 1) * 512)
                for k in range(8):
                    B.mm(p[:], hT[:, k, :], Wm[:, k, cs], k == 0, k == 7, [hk, ("Wm", k)], [pk])
                B.tt("dve", tmpg[:], p[:], bmb[:, cs], ALU.add, [pk, "bmb"], ["tmpg"])
                B.act(gt[:, cs], tmpg[:], AF.Sigmoid, ["tmpg"], [("gt", cg)])
            for k in range(4):
                B.tr(fr.pT[:, k, :], ya[:, t, k * 128:(k + 1) * 128], identb[:], [("ya", t), "identb"], ["pT"])
            for k in range(4):
                B.tr(fr.pT[:, 4 + k, :], yb[:, t, k * 128:(k + 1) * 128], identb[:], [("yb", t), "identb"], ["pT"])
            B.cp("act", yT[:], fr.pT[:], ["pT"], ["yT"])
            for half in range(2):
                hs_ = slice(half * 512, (half + 1) * 512)
                pk = "pa%d" % half
                for k in range(4):
                    B.mm(pa[half][:], yT[:, k, :], wua[:, k, hs_], k == 0, k == 3, ["yT", ("wua", k)], [pk])
                B.tt("dve", m1[:, hs_], pa[half][:], gt[:, hs_], ALU.mult, [pk, ("gt", half)], [("m1", half)])
            for half in range(2):
                hs_ = slice(half * 512, (half + 1) * 512)
                pk = "pa%d" % half
                for k in range(4):
                    B.mm(pa[half][:], yT[:, 4 + k, :], wub[:, k, hs_], k == 0, k == 3, ["yT", ("wub", k)], [pk])
                B.tt("dve", tmpg[:], pa[half][:], gt[:, 1024 + half * 512:1024 + (half + 1) * 512], ALU.mult,
                     [pk, ("gt", 2 + half)], ["tmpg"])
                B.tt("dve", mg[:, hs_], tmpg[:], m1[:, hs_], ALU.add, ["tmpg", ("m1", half)], [("mg", half)])
            for k in range(8):
                B.tr(fr.pT[:, k, :], mg[:, k * 128:(k + 1) * 128], identb[:], [("mg", k // 4), "identb"], ["pT"])
            B.cp("act", mT[:], fr.pT[:], ["pT"], ["mT"])
            x2 = x2t[t % 2]
            for half in range(2):
                hs_ = slice(half * 512, (half + 1) * 512)
                pk = "pa%d" % half
                for k in range(8):
                    B.mm(pa[half][:], mT[:, k, :], wo[:, k, hs_], k == 0, k == 7, ["mT", ("wo", k)], [pk])
                B.tt("dve", x2[:, hs_], pa[half][:], xt[:, hs_], ALU.add, [pk, xk], [("x2t", t % 2, half)])
            B.dma("sp", x2_d[t * 128:(t + 1) * 128, :], x2[:], [("x2t", t % 2, 0), ("x2t", t % 2, 1)], [("x2d", t)])
    B.S.barrier()
    attn_stack.close()
    B.dump("x2", x2_d[0:NT_RUN * 128, :], [("x2d", t) for t in range(NT_RUN)], [NT_RUN * 128, DM])
    stop("M")

    with B.scope():
        n2g = B.sb("n2g", [128, DM])
        B.dma("sp", n2g[:], bcast_rows(n2g_d, 128, DM), [], ["n2g"])
        wq = B.sb("wq", [128, 8, 2048], BF16)
        for k in range(8):
            B.dma("pool", wq[:, k, :], wq_d[k * 128:(k + 1) * 128, :], [], [("wq", k)])
        kT = [B.sb("k1T", [128, 128], BF16), B.sb("k2T", [128, 128], BF16)]
        B.dma("pool", kT[0][:], k1T_d.ap(), [], ["k1T"])
        B.dma("pool", kT[1][:], k2T_d.ap(), [], ["k2T"])
        ktok = ["k1T", "k2T"]
        h2Tg = B.sb("h2Tg", [128, 8, 512], BF16)
        eaz = B.sb("eaz", [128, 4, 8, 128])
        ebz = B.sb("ebz", [128, 4, 8, 128])
        thp = B.sb("thp", [128, 4, 8])
        WTs = [B.sb("WT", [128, 16, 512], BF16) for _ in range(2)]
        oaccT = B.sb("oaccT", [128, 8, 512])
        pT = B.ps("pT", [128, 8, 128], BF16)
        pq = [B.ps("pq", [128, 512], tok="pq%d" % i) for i in range(2)]
        acc = [B.ps("acc", [128, 512], tok="acc%d" % i) for i in range(4)]
        uTv = uT_d.ap().rearrange("(k p) e -> p k e", p=128)
        NG = max(1, NT_RUN // 4) if PEER_GROUPS is None else PEER_GROUPS
        for G in range(NG):
            h2toks = [("h2Tg", tt) for tt in range(4)]
            with B.scope():
                hb2 = B.sb("hb2", [128, DM], BF16)
                x2r = [B.sb("x2r", [128, DM]) for _ in range(2)]
                ss = B.sb("ss", [128, 1])
                rs = B.sb("rs", [128, 1])
                qTs = B.sb("qTs", [128, 16, 512], BF16)
                s12 = [B.sb("s1", [128, 8, 128]), B.sb("s2", [128, 8, 128])]
                v12 = [B.sb("v1", [128, 8, 16]), B.sb("v2", [128, 8, 16])]
                tmpm = B.sb("tmpm", [128, 128])
                cand = B.sb("cand", [128, 8, 256])
                sel = B.sb("sel", [128, 8, 256])
                c8a = B.sb("c8a", [128, 8, 8])
                c8b = B.sb("c8b", [128, 8, 8])
                tmp256 = B.sb("tmp256", [128, 256])
                Zt = B.sb("Zt", [128, 8])
                tm = B.sb("tm", [128, 8])
                for tt in range(4):
                    t = G * 4 + tt
                    x2c = x2r[tt % 2]
                    xk2 = ("x2r", tt % 2)
                    B.dma("sp", x2c[:], x2_d[t * 128:(t + 1) * 128, :], [("x2d", t)], [xk2])
                    B.act(hb2[:], x2c[:], AF.Square, [xk2], ["hb2", "ss"], accum=ss[:])
                    B.act(rs[:], ss[:], AF.Sqrt, ["ss", "epsb"], ["rs"], bias=epsb[:], scale=1.0 / DM)
                    B.recip(rs[:], rs[:], ["rs"], ["rs"])
                    B.stt("dve", hb2[:], x2c[:], rs[:], n2g[:], ALU.mult, ALU.mult, [xk2, "rs", "n2g"], ["hb2"])
                    for k in range(8):
                        B.tr(pT[:, k, :], hb2[:, k * 128:(k + 1) * 128], identb[:], ["hb2", "identb"], ["pT"])
                    B.cp("act", h2Tg[:, :, tt * 128:(tt + 1) * 128], pT[:], ["pT"], [("h2Tg", tt)])
                for cc in range(16):
                    p = pq[cc % 2]
                    pk = "pq%d" % (cc % 2)
                    for k in range(8):
                        B.mm(p[:], wq[:, k, cc * 128:(cc + 1) * 128], h2Tg[:, k, :], k == 0, k == 7, [("wq", k)] + h2toks, [pk])
                    B.cp("act", qTs[:, cc, :], p[:], [pk], [("qTs", cc)])
                for tt in range(4):
                    tsl = slice(tt * 128, (tt + 1) * 128)
                    for side in range(2):
                        for hh in range(2):
                            bi = (side * 2 + hh) % 2
                            p = pq[bi]
                            pk = "pq%d" % bi
                            for j in range(4):
                                h = hh * 4 + j
                                B.mm(p[:, j * 128:(j + 1) * 128], qTs[:, 2 * h + side, tsl], kT[side][:], True, True,
                                     [("qTs", 2 * h + side), ktok[side]], [pk])
                            B.cp("act", s12[side][:, hh * 4:(hh + 1) * 4, :], p[:].rearrange("p (j n) -> p j n", n=128),
                                 [pk], [("s", side, hh)])
                    for side in range(2):
                        st_ = [("s", side, 0), ("s", side, 1)]
                        vk = ("v", side)
                        for h in range(8):
                            B.max8(v12[side][:, h, 0:8], s12[side][:, h, :], st_, [vk])
                            B.mrep(tmpm[:], v12[side][:, h, 0:8], s12[side][:, h, :], -3.0e38, st_ + [vk], ["tmpm"])
                            B.max8(v12[side][:, h, 8:16], tmpm[:], ["tmpm"], [vk])
                    v1b = bass.AP(v12[0], 0, [[128, 128], [16, 8], [1, 16], [0, 16]])
                    v2b = bass.AP(v12[1], 0, [[128, 128], [16, 8], [0, 16], [1, 16]])
                    B.tt("dve", cand[:].rearrange("p h (r c) -> p h r c", c=16), v1b, v2b, ALU.add, [("v", 0), ("v", 1)], ["cand"])
                    for h in range(8):
                        B.max8(c8a[:, h, :], cand[:, h, :], ["cand"], ["c8a"])
                        B.mrep(tmp256[:], c8a[:, h, :], cand[:, h, :], -3.0e38, ["cand", "c8a"], ["tmp256"])
                        B.max8(c8b[:, h, :], tmp256[:], ["tmp256"], ["c8b"])
                    taub = bass.AP(c8b, 7, [[64, 128], [8, 8], [0, 256]])
                    mb_ = bass.AP(c8a, 0, [[64, 128], [8, 8], [0, 256]])
                    B.tt("dve", sel[:], cand[:], taub, ALU.is_ge, ["cand", "c8b"], ["sel"])
                    B.tt("dve", cand[:], cand[:], mb_, ALU.subtract, ["cand", "c8a"], ["cand"])
                    B.act(cand[:], cand[:], AF.Exp, ["cand"], ["cand"])
                    B.tt("dve", sel[:], sel[:], cand[:], ALU.mult, ["sel", "cand"], ["sel"])
                    B.red(Zt[:], sel[:], ALU.add, ["sel"], ["Zt"])
                    B.recip(Zt[:], Zt[:], ["Zt"], ["Zt"])
                    rZb = bass.AP(Zt, 0, [[8, 128], [1, 8], [0, 128]])
                    m1b = bass.AP(v12[0], 0, [[128, 128], [16, 8], [0, 128]])
                    m2b = bass.AP(v12[1], 0, [[128, 128], [16, 8], [0, 128]])
                    ek = ("eaz", tt)
                    bk = ("ebz", tt)
                    B.tt("dve", eaz[:, tt], s12[0][:], m1b, ALU.subtract, [("s", 0, 0), ("s", 0, 1), ("v", 0)], [ek])
                    B.act(eaz[:, tt], eaz[:, tt], AF.Exp, [ek], [ek])
                    B.tt("dve", eaz[:, tt], eaz[:, tt], rZb, ALU.mult, [ek, "Zt"], [ek])
                    B.tt("dve", ebz[:, tt], s12[1][:], m2b, ALU.subtract, [("s", 1, 0), ("s", 1, 1), ("v", 1)], [bk])
                    B.act(ebz[:, tt], ebz[:, tt], AF.Exp, [bk], [bk])
                    B.tt("dve", tm[:], c8b[:, :, 7], c8a[:, :, 0], ALU.subtract, ["c8a", "c8b"], ["tm"])
                    B.ts("dve", tm[:], tm[:], -4.0e-6, None, ALU.add, None, ["tm"], ["tm"])
                    B.act(tm[:], tm[:], AF.Exp, ["tm"], ["tm"])
                    B.tt("dve", thp[:, tt, :], tm[:], Zt[:], ALU.mult, ["tm", "Zt"], [("thp", tt)])
            with B.scope():
                Ebufs = [B.sb("Ebuf", [128, 16, 128]) for _ in range(2)]
                Waccs = [B.sb("Wacc", [128, 2048], BF16) for _ in range(2)]
                Whs = [B.sb("Wh", [128, 2048], BF16) for _ in range(2)]
                Ub = [B.sb("Ub", [128, 8, 512], BF16) for _ in range(2)]
                Vb = [B.sb("Vb", [128, 4, 512], BF16) for _ in range(2)]
                Gb = [B.sb("Gb", [128, 512], BF16) for _ in range(2)]
                outt = [B.sb("outt", [128, DM])] * 2
                tacc = B.sb("tacc", [128, 8, 512])
                ui = 0
                vi = 0
                gbi = 0
                ei = 0
                wi = 0
                pending = []

                def build_tt(st, tt):
                    nonlocal ei, wi
                    WT = WTs[st % 2]
                    Wacc = Waccs[wi % 2]
                    wak = ("Wacc", wi % 2)
                    wi += 1
                    stages = []
                    for h in range(8):
                        Ebuf = Ebufs[ei % 2]
                        ebk = ("Ebuf", ei % 2)
                        whb = Whs[ei % 2]
                        whk = ("Wh", ei % 2)
                        ei += 1
                        Ef = Ebuf[:].rearrange("p i j -> p (i j)")
                        ea_ap = bass.AP(eaz, (tt * 8 + h) * 128 + 16 * st, [[4096, 128], [1, 16], [0, 128]])
                        eb_ap = bass.AP(ebz, (tt * 8 + h) * 128, [[4096, 128], [0, 16], [1, 128]])
                        th = thp[:, tt, h:h + 1]
                        if h in (3, 7):
                            f_e = (lambda Ebuf=Ebuf, ea_ap=ea_ap, eb_ap=eb_ap, ebk=ebk:
                                   B.tt("dve", Ebuf[:], ea_ap, eb_ap, ALU.mult, [("eaz", tt), ("ebz", tt)], [ebk]))
                        else:
                            def f_e(Ebuf=Ebuf, ebk=ebk, h=h):
                                for i_ in range(16):
                                    sc_ap = eaz[:, tt, h, 16 * st + i_:16 * st + i_ + 1]
                                    S.add("act", lambda e, o_=Ebuf[:, i_, :], in__=ebz[:, tt, h, :], sc_=sc_ap:
                                          e.activation(out=o_, in_=in__, func=AF.Copy, scale=sc_),
                                          [("eaz", tt), ("ebz", tt)], [ebk])
                        if h == 0:
                            f_s = (lambda Ef=Ef, th=th, ebk=ebk:
                                   B.stt("dve", Wacc[:], Ef, th, Ef, ALU.is_ge, ALU.mult, [ebk, ("thp", tt)], [wak]))
                            f_a = None
                        else:
                            f_s = (lambda Ef=Ef, th=th, ebk=ebk, whb=whb, whk=whk:
                                   B.stt("dve", whb[:], Ef, th, Ef, ALU.is_ge, ALU.mult, [ebk, ("thp", tt)], [whk]))
                            f_a = (lambda whb=whb, whk=whk:
                                   B.tt("dve", Wacc[:], Wacc[:], whb[:], ALU.add, [wak, whk], [wak]))
                        stages.append((f_e, f_s, f_a))
                    for k_ in range(8 + 2):
                        if k_ < 8:
                            stages[k_][0]()
                        if 0 <= k_ - 1 < 8:
                            stages[k_ - 1][1]()
                        if 0 <= k_ - 2 < 8 and stages[k_ - 2][2] is not None:
                            stages[k_ - 2][2]()
                    for b2 in range(2):
                        for j in range(8):
                            ec = b2 * 8 + j
                            B.tr(pT[:, j, :], Wacc[:, ec * 128:(ec + 1) * 128], identb[:], [wak, "identb"], ["pT"])
                        B.cp("act", WT[:, b2 * 8:(b2 + 1) * 8, tt * 128:(tt + 1) * 128], pT[:], ["pT"],
                             [("WT", st % 2, ec_) for ec_ in range(b2 * 8, b2 * 8 + 8)])

                def gemm1(st):
                    nonlocal ui, gbi
                    WT = WTs[st % 2]
                    for e4 in range(4):
                        E0 = st * 16 + e4 * 4
                        ub = Ub[ui % 2]
                        uk = ("Ub", ui % 2)
                        ui += 1
                        B.dma("pool", ub[:], uTv[:, :, E0 * 128:E0 * 128 + 512], [], [uk])
                        for j in range(4):
                            ec = e4 * 4 + j
                            p = pq[ec % 2]
                            pk = "pq%d" % (ec % 2)
                            for k in range(8):
                                B.mm(p[:], ub[:, k, j * 128:(j + 1) * 128], h2Tg[:, k, :], k == 0, k == 7, [uk] + h2toks, [pk])
                            gb_ = Gb[gbi % 2]
                            gk = ("Gb", gbi % 2)
                            gbi += 1
                            B.act(gb_[:], p[:], AF.Gelu_apprx_tanh, [pk], [gk])
                            B.tt("pool", WT[:, ec, :], WT[:, ec, :], gb_[:], ALU.mult, [("WT", st % 2, ec), gk], [("WT", st % 2, ec)])

                def gemm2(st, dh):
                    nonlocal vi
                    WT = WTs[st % 2]
                    for e4 in range(4):
                        E0 = st * 16 + e4 * 4
                        vb = Vb[vi % 2]
                        vk_ = ("Vb", vi % 2)
                        vi += 1
                        B.dma("pool", vb[:], pv_d[E0 * 128:E0 * 128 + 512, dh * 512:(dh + 1) * 512].rearrange("(c p) d -> p c d", p=128),
                              [], [vk_])
                        for j in range(4):
                            ec = e4 * 4 + j
                            for dk in range(4):
                                B.mm(acc[dk][:], vb[:, j, dk * 128:(dk + 1) * 128], WT[:, ec, :], ec == 0, ec == 15,
                                     [vk_, ("WT", st % 2, ec)], ["acc%d" % dk])
                    for dk in range(4):
                        dki = dh * 4 + dk
                        if st == 0:
                            B.cp("act", oaccT[:, dki, :], acc[dk][:], ["acc%d" % dk], [("oaccT", dki)])
                        else:
                            B.cp("act", tacc[:, dki, :], acc[dk][:], ["acc%d" % dk], [("tacc", dki)])
                            pending.append(lambda dki=dki: B.tt(
                                "dve", oaccT[:, dki, :], oaccT[:, dki, :], tacc[:, dki, :], ALU.add,
                                [("tacc", dki), ("oaccT", dki)], [("oaccT", dki)]))

                for st in range(8):
                    ready = pending
                    pending = []
                    for f_ in ready:
                        f_()
                    chunks = []
                    if st > 0:
                        chunks = [lambda: gemm1(st - 1), lambda: gemm2(st - 1, 0), lambda: gemm2(st - 1, 1)]
                    for tt in range(4):
                        build_tt(st, tt)
                        if tt < len(chunks):
                            chunks[tt]()
                ready = pending
                pending = []
                for f_ in ready:
                    f_()
                gemm1(7)
                gemm2(7, 0)
                gemm2(7, 1)
                for f_ in pending:
                    f_()
                pending = []
                for tt in range(4):
                    t = G * 4 + tt
                    ot = outt[tt % 2]
                    B.dma("sp", ot[:], x2_d[t * 128:(t + 1) * 128, :], [("x2d", t)], [("outt", 0, 0), ("outt", 0, 1)])
                    for half in range(2):
                        p = pq[half]
                        pk = "pq%d" % half
                        for j in range(4):
                            dk = half * 4 + j
                            B.tr(p[:, j * 128:(j + 1) * 128], oaccT[:, dk, tt * 128:(tt + 1) * 128], ident[:],
                                 [("oaccT", dk), "ident"], [pk])
                        B.tt("dve", ot[:, half * 512:(half + 1) * 512], p[:], ot[:, half * 512:(half + 1) * 512], ALU.add,
                             [pk, ("outt", 0, half)], [("outt", 0, half)])
                    B.dma("sp", out_d[t * 128:(t + 1) * 128, :], ot[:], [("outt", 0, 0), ("outt", 0, 1)], [("outd", t)])


def finish(nc, B):
    run = B.S.emit(nc, B.root.enter_context)
    with nc.Block() as block:
        @block.sync
        def _(e):
            run("sp", e, final=True)

        @block.tensor
        def _(e):
            run("pe", e)

        @block.scalar
        def _(e):
            run("act", e)

        @block.vector
        def _(e):
            run("dve", e)

        @block.gpsimd
        def _(e):
            run("pool", e)
    B.root.close()


def host_inputs(inp, b, shared=None):
    w_in = inp["w_in"][0]
    d = {
        "x": np.ascontiguousarray(inp["x"][b]),
        "w_a": np.ascontiguousarray(w_in[:, 0:1536]),
        "norm1_g": np.ascontiguousarray(inp["norm1_g"]),
        "a_q_g": np.ascontiguousarray(inp["a_q_g"]),
        "a_k_g": np.ascontiguousarray(inp["a_k_g"]),
    }
    c = lambda a: np.ascontiguousarray(a, dtype=np.float32)
    o = 1536
    bq = [w_in[:, o + (g * 4 + r) * 64: o + (g * 4 + r + 1) * 64] for r in range(4) for g in range(2)]
    bkc, bvc, bks, bvs, bkw, bvw = [w_in[:, 2048 + i * 128: 2048 + (i + 1) * 128] for i in range(6)]
    bgate = w_in[:, 2816:2840]
    d["w_b"] = c(np.concatenate(bq + [bks, bkw, bkc, bvc, bvs, bvw, bgate], axis=1))
    d["w_m"] = c(w_in[:, 2840:4888])
    for k in ("b_q_g", "b_kc_g", "b_ks_g", "b_kw_g"):
        d[k] = c(inp[k])
    for kv in ("k", "v"):
        d[f"cmp_pos_{kv}T"] = c(np.repeat(inp[f"cmp_pos_{kv}"][0].T, 2, axis=1))
        d[f"cmp_{kv}_w1"] = c(inp[f"cmp_{kv}_w1"][0])
        d[f"cmp_{kv}_b1"] = c(inp[f"cmp_{kv}_b1"][0].reshape(2, 128).T)
        d[f"cmp_{kv}_w2"] = c(inp[f"cmp_{kv}_w2"][0])
        d[f"cmp_{kv}_b2"] = c(inp[f"cmp_{kv}_b2"])
    d["b_merge"] = c(inp["b_merge"][0].reshape(1, 2048))
    d["w_up_a"] = c(inp["w_up_a"][0])
    d["w_up_b"] = c(inp["w_up_b"][0])
    d["w_out"] = c(inp["w_out"][0])
    d["norm2_g"] = c(inp["norm2_g"])
    d["peer_wq"] = c(inp["peer_wq"][0])
    d["peer_k1T"] = c(inp["peer_k1"][0].T)
    d["peer_k2T"] = c(inp["peer_k2"][0].T)
    d["peer_uT"] = shared["uT"] if shared is not None else c(inp["peer_u"][0].T)
    d["peer_v"] = shared["v"] if shared is not None else c(inp["peer_v"][0])
    return d


def kernel(**inputs):
    nc, B = build_program()
    finish(nc, B)
    shared = {"uT": np.ascontiguousarray(inputs["peer_u"][0].T, dtype=np.float32),
              "v": np.ascontiguousarray(inputs["peer_v"][0], dtype=np.float32)}
    in_maps = [host_inputs(inputs, b, shared) for b in range(8)]
    res = run_bass_kernel_spmd(nc, in_maps, core_ids=list(range(8)))
    return np.stack([np.asarray(r["out"]) for r in res.results], 0).astype(np.float32)
```

```python
from contextlib import ExitStack
import math
import numpy as np
import concourse.bass as bass
import concourse.mybir as mybir
from concourse.bass_utils import run_bass_kernel_spmd

F32 = mybir.dt.float32
BF16 = mybir.dt.bfloat16
ALU = mybir.AluOpType
AF = mybir.ActivationFunctionType
AX = mybir.AxisListType

S_LEN = 4096
NT = 32
NT_RUN = 32
SKIP = set()
QT_LIST = None
PEER_GROUPS = None
DM = 1024
NEG = -30000.0
BIGF = 1e30
EPS = 1e-6
SCALE = 0.125

ENGS = ("pe", "act", "dve", "pool", "sp")
EPOCH = 20000
NDMA = {"sp": 16, "act": 8, "pool": 12}


class _Op:
    __slots__ = ("eng", "fn", "dma", "gid", "deps", "signal", "sem", "val", "pre", "drain")

    def __init__(self, eng, fn, dma):
        self.eng = eng
        self.fn = fn
        self.dma = dma
        self.deps = ()
        self.signal = False
        self.sem = None
        self.val = 0
        self.pre = None
        self.drain = False


class Sched:
    def __init__(self):
        self.ops = []
        self.lastw = {}
        self.readers = {}
        self.last_op = {}
        self.excl = set()

    def add(self, eng, fn, reads=(), writes=(), dma=False):
        if self.excl:
            ex = [t for t in reads if t in self.excl]
            if ex:
                writes = list(writes) + [t for t in ex if t not in writes]
        op = _Op(eng, fn, dma)
        op.gid = len(self.ops)
        ops = self.ops
        keep = set()

        def consider(kind, d):
            dop = ops[d]
            if dop.dma:
                keep.add(d)
                return
            if dop.eng == eng and not dma and eng == "pe":
                return
            keep.add(d)

        for t in reads:
            w = self.lastw.get(t)
            if w is not None:
                consider("raw", w)
        for t in writes:
            w = self.lastw.get(t)
            if w is not None:
                consider("waw", w)
            rd = self.readers.get(t)
            if rd:
                for r in rd.values():
                    for g in r:
                        consider("war", g)
        op.deps = keep
        for t in reads:
            rd = self.readers.setdefault(t, {})
            if dma:
                rd.setdefault("dma", []).append(op.gid)
            else:
                rd[eng] = [op.gid]
        for t in writes:
            self.lastw[t] = op.gid
            self.readers[t] = {}
        if not dma:
            self.last_op[eng] = op.gid
        ops.append(op)
        return op

    def barrier(self):
        a_ids = []
        for e in ENGS:
            op = self.add(e, lambda h: h.nop(), writes=[("barA", e)])
            op.drain = True
            lo = None
            for g in range(op.gid - 1, -1, -1):
                o = self.ops[g]
                if o.eng == e and not o.dma:
                    lo = g
                    break
            if lo is not None:
                op.deps = set(op.deps) | {lo}
            a_ids.append(op.gid)
        for e in ENGS:
            op = self.add(e, lambda h: h.nop(), writes=[("barB", e)])
            op.deps = set(op.deps) | set(a_ids)
        self.lastw.clear()
        self.readers.clear()

    def emit(self, nc, enter):
        ops = self.ops
        for op in ops:
            for d in op.deps:
                ops[d].signal = True
        ncnt = {e: 0 for e in ENGS}
        ndma = {e: 0 for e in ENGS}
        for op in ops:
            if op.dma:
                ndma[op.eng] += 1
            elif op.signal:
                ncnt[op.eng] += 1
        sems = {}
        for e in ENGS:
            n_ep = max(1, (ncnt[e] + EPOCH - 1) // EPOCH)
            sems[e] = [enter(nc.semaphore(f"s_{e}{k}")) for k in range(n_ep)]
        dsems = {}
        for e in ENGS:
            if ndma[e]:
                dsems[e] = [enter(nc.semaphore(f"d_{e}{k}")) for k in range(min(NDMA.get(e, 8), ndma[e]))]
        cnt = {e: 0 for e in ENGS}
        dcnt = {e: 0 for e in ENGS}
        for op in ops:
            e = op.eng
            if op.dma:
                i = dcnt[e]
                dcnt[e] += 1
                n = len(dsems[e])
                op.sem = dsems[e][i % n]
                op.val = 16 * (i // n + 1)
                op.pre = (op.sem, 16 * (i // n)) if i >= n else None
            elif op.signal:
                k = cnt[e]
                cnt[e] += 1
                op.sem = sems[e][k // EPOCH]
                op.val = k % EPOCH + 1
        per_eng = {e: [op for op in ops if op.eng == e] for e in ENGS}

        def run_engine(ename, handle, final=False):
            waited = {}
            outstanding = {}

            def wait(sem, val):
                key = id(sem)
                if waited.get(key, 0) < val:
                    handle.wait_ge(sem, val)
                    waited[key] = val

            for op in per_eng[ename]:
                for d in sorted(op.deps):
                    dop = ops[d]
                    wait(dop.sem, dop.val)
                if op.pre is not None:
                    wait(*op.pre)
                if op.drain:
                    for (sem, val) in outstanding.values():
                        wait(sem, val)
                inst = op.fn(handle)
                if op.dma:
                    inst.then_inc(op.sem, 16)
                    outstanding[id(op.sem)] = (op.sem, op.val)
                elif op.signal:
                    inst.then_inc(op.sem, 1)
            if final:
                for (sem, val) in outstanding.values():
                    wait(sem, val)

        return run_engine


class Builder:
    def __init__(self, nc, dbg=False):
        self.nc = nc
        self.S = Sched()
        self.dbg = dbg
        self.dbg_outs = []
        self.root = ExitStack()
        self.stack = self.root
        self._uid = 0

    def sb(self, name, shape, dt=F32):
        self._uid += 1
        return self.stack.enter_context(self.nc.sbuf_tensor(f"{name}_{self._uid}", list(shape), dt))

    def ps(self, name, shape, dt=F32, tok=None):
        self._uid += 1
        nbytes = int(np.prod(shape[1:])) * (4 if dt == F32 else 2)
        assert nbytes == 2048, (name, shape)
        self.S.excl.add(tok if tok is not None else name)
        return self.stack.enter_context(self.nc.psum_tensor(f"{name}_{self._uid}", list(shape), dt))

    def scope(self):
        b = self

        class _Sc:
            def __enter__(s):
                s.prev = b.stack
                s.st = ExitStack()
                b.stack = s.st
                return s

            def __exit__(s, *a):
                b.S.barrier()
                s.st.close()
                b.stack = s.prev
                return False

        return _Sc()

    def mm(self, out, lhsT, rhs, start, stop, r, w):
        self.S.add("pe", lambda e: e.matmul(out, lhsT=lhsT, rhs=rhs, start=start, stop=stop), r, w)

    def tr(self, out, in_, ident, r, w):
        self.S.add("pe", lambda e: e.transpose(out=out, in_=in_, identity=ident), r, w)

    def act(self, out, in_, func, r, w, bias=None, scale=None, accum=None):
        kw = {}
        if bias is not None:
            kw["bias"] = bias
        if scale is not None:
            kw["scale"] = scale
        if accum is not None:
            kw["accum_out"] = accum
        self.S.add("act", lambda e: e.activation(out=out, in_=in_, func=func, **kw), r, w)

    def tt(self, eng, out, a, b, op, r, w):
        self.S.add(eng, lambda e: e.tensor_tensor(out=out, in0=a, in1=b, op=op), r, w)

    def ts(self, eng, out, a, s1, s2, op0, op1, r, w):
        if op1 is None:
            self.S.add(eng, lambda e: e.tensor_scalar(out=out, in0=a, scalar1=s1, scalar2=None, op0=op0), r, w)
        else:
            self.S.add(eng, lambda e: e.tensor_scalar(out=out, in0=a, scalar1=s1, scalar2=s2, op0=op0, op1=op1), r, w)

    def stt(self, eng, out, a, scalar, b, op0, op1, r, w):
        self.S.add(eng, lambda e: e.scalar_tensor_tensor(out=out, in0=a, scalar=scalar, in1=b, op0=op0, op1=op1), r, w)

    def cp(self, eng, out, in_, r, w):
        if eng == "act":
            self.S.add("act", lambda e: e.copy(out=out, in_=in_), r, w)
        else:
            self.S.add(eng, lambda e: e.tensor_copy(out=out, in_=in_), r, w)

    def red(self, out, in_, op, r, w, eng="dve"):
        self.S.add(eng, lambda e: e.tensor_reduce(out=out, in_=in_, axis=AX.X, op=op), r, w)

    def recip(self, out, in_, r, w):
        self.S.add("dve", lambda e: e.reciprocal(out=out, in_=in_), r, w)

    def memset(self, eng, ap, val, w):
        self.S.add(eng, lambda e: e.memset(ap, val), (), w)

    def asel(self, out, in_, pattern, cmp, fill, base, cm, r, w):
        self.S.add("pool", lambda e: e.affine_select(out=out, in_=in_, pattern=pattern, compare_op=cmp, fill=fill,
                                                     base=base, channel_multiplier=cm), r, w)

    def dma(self, eng, out, in_, r, w):
        self.S.add(eng, lambda e: e.dma_start(out=out, in_=in_), r, w, dma=True)

    def max8(self, out, in_, r, w):
        self.S.add("dve", lambda e: e.max(out=out, in_=in_), r, w)

    def mrep(self, out, rep, vals, imm, r, w):
        self.S.add("dve", lambda e: e.match_replace(out=out, in_to_replace=rep, in_values=vals, imm_value=imm), r, w)

    def dump(self, name, ap, tok, shape, dt=F32):
        if not self.dbg:
            return
        d = self.nc.dram_tensor("dbg_" + name, list(shape), dt, kind="ExternalOutput")
        self.dbg_outs.append("dbg_" + name)
        self.dma("sp", d.ap(), ap, [tok] if not isinstance(tok, list) else tok, [("dbgo", name)])


def bcast_rows(t, n, width, off=0):
    return bass.AP(t, off, [[0, n], [1, width]])


class StopBuild(Exception):
    pass


def build_program(dbg=False, stop_after=None):
    nc = bass.Bass("TRN2", target_bir_lowering=False)
    B = Builder(nc, dbg)
    try:
        _build(nc, B, dbg, stop_after)
    except StopBuild:
        pass
    return nc, B


def _build(nc, B, dbg, stop_after):
    def stop(name):
        if stop_after == name:
            raise StopBuild()

    S = B.S
    D = {}

    def din(name, shape):
        D[name] = nc.dram_tensor(name, list(shape), F32, kind="ExternalInput")
        return D[name]

    x_d = din("x", [S_LEN, DM])
    wa_d = din("w_a", [DM, 1536])
    n1g_d = din("norm1_g", [1, DM])
    aqg_d = din("a_q_g", [1, 64])
    akg_d = din("a_k_g", [1, 64])
    wb_d = din("w_b", [DM, 1304])
    wm_d = din("w_m", [DM, 2048])
    bqg_d = din("b_q_g", [1, 64])
    bkcg_d = din("b_kc_g", [1, 64])
    bksg_d = din("b_ks_g", [1, 64])
    bkwg_d = din("b_kw_g", [1, 64])
    cmp_d = {}
    for kv in ("k", "v"):
        cmp_d[kv] = dict(
            posT=din(f"cmp_pos_{kv}T", [64, 64]), w1=din(f"cmp_{kv}_w1", [2048, 256]),
            b1=din(f"cmp_{kv}_b1", [128, 2]), w2=din(f"cmp_{kv}_w2", [256, 64]), b2=din(f"cmp_{kv}_b2", [1, 64]))
    bm_d = din("b_merge", [1, 2048])
    wua_d = din("w_up_a", [512, DM])
    wub_d = din("w_up_b", [512, DM])
    wo_d = din("w_out", [DM, DM])
    n2g_d = din("norm2_g", [1, DM])
    wq_d = din("peer_wq", [DM, 2048])
    k1T_d = din("peer_k1T", [128, 128])
    k2T_d = din("peer_k2T", [128, 128])
    uT_d = din("peer_uT", [DM, 16384])
    pv_d = din("peer_v", [16384, DM])
    x2_d = nc.dram_tensor("x2_scratch", [S_LEN, DM], F32)
    out_d = nc.dram_tensor("out", [S_LEN, DM], F32, kind="ExternalOutput")

    ident = B.sb("ident", [128, 128])
    identb = B.sb("identb", [128, 128], BF16)
    B.memset("pool", ident[:], 0.0, ["ident"])
    B.asel(ident[:], ident[:], [[-1, 128]], ALU.not_equal, 1.0, 0, 1, ["ident"], ["ident"])
    B.cp("dve", identb[:], ident[:], ["ident"], ["identb"])
    causT = B.sb("causT", [128, 128], BF16)
    B.memset("pool", causT[:], 0.0, ["causT"])
    B.asel(causT[:], causT[:], [[1, 128]], ALU.is_ge, NEG, 0, -1, ["causT"], ["causT"])
    epsb = B.sb("epsb", [128, 1])
    B.memset("dve", epsb[:], EPS, ["epsb"])
    negpi = B.sb("negpi", [128, 1])
    B.memset("dve", negpi[:], -math.pi, ["negpi"])
    attn_stack = ExitStack()
    _prev_stack = B.stack
    B.stack = attn_stack
    cosT = B.sb("cosT", [128, NT, 8])
    sinT = B.sb("sinT", [128, NT, 8])
    n1g = B.sb("n1g", [128, DM])
    antiT = B.sb("antiT", [128, 128], BF16)
    B.stack = _prev_stack
    with B.scope():
        pos = B.sb("pos", [128, NT])
        S.add("pool", lambda e: e.iota(pos[:], pattern=[[128, NT]], base=0, channel_multiplier=1,
                                       allow_small_or_imprecise_dtypes=True), (), ["pos"])
        ang = B.sb("ang", [128, NT, 8])
        for i in range(8):
            inv = float(np.float32(500000.0) ** np.float32(-(2.0 * i) / 16.0))
            B.ts("dve", ang[:, :, i], pos[:], inv, None, ALU.mult, None, ["pos"], ["ang"])
        ki = B.sb("ki", [128, NT, 8], mybir.dt.int32)
        kf = B.sb("kf", [128, NT, 8])
        rr = B.sb("rr", [128, NT, 8])
        for tab, shift in ((sinT, 0.0), (cosT, 0.5 * math.pi)):
            B.ts("dve", rr[:], ang[:], shift, 1.0 / (2 * math.pi), ALU.add, ALU.mult, ["ang"], ["rr"])
            B.cp("dve", ki[:], rr[:], ["rr"], ["ki"])
            B.cp("dve", kf[:], ki[:], ["ki"], ["kf"])
            B.ts("dve", kf[:], kf[:], -2 * math.pi, None, ALU.mult, None, ["kf"], ["kf"])
            B.stt("dve", rr[:], ang[:], shift, kf[:], ALU.add, ALU.add, ["ang", "kf"], ["rr"])
            B.ts("dve", kf[:], rr[:], math.pi, -2 * math.pi, ALU.is_gt, ALU.mult, ["rr"], ["kf"])
            B.tt("dve", rr[:], rr[:], kf[:], ALU.add, ["rr", "kf"], ["rr"])
            B.ts("dve", kf[:], rr[:], -math.pi, 2 * math.pi, ALU.is_lt, ALU.mult, ["rr"], ["kf"])
            B.tt("dve", rr[:], rr[:], kf[:], ALU.add, ["rr", "kf"], ["rr"])
            B.act(tab[:], rr[:], AF.Sin, ["rr"], ["cosT" if shift > 0 else "sinT", "tab%d" % (shift > 0)])
    B.dma("sp", n1g[:], bcast_rows(n1g_d, 128, DM), [], ["n1g"])

    B.dump("cosT", cosT[:].rearrange("p t i -> p (t i)"), "tab1", [128, NT * 8])
    B.dump("sinT", sinT[:].rearrange("p t i -> p (t i)"), "tab0", [128, NT * 8])
    B.dump("n1g", n1g[:], "n1g", [128, DM])
    stop("C")

    def rope_ap(tab, t, nh):
        return bass.AP(tab, t * 8, [[NT * 8, 128], [0, nh], [1, 8]])

    class Front:
        def __init__(self):
            self.xt = [B.sb("xt", [128, DM]) for _ in range(2)]
            self.ss = B.sb("ss", [128, 1])
            self.rs = B.sb("rs", [128, 1])
            self.hb = B.sb("hb", [128, DM], BF16)
            self.hT = [B.sb("hT", [128, 8, 128], BF16) for _ in range(2)]
            self.pT = B.ps("pT", [128, 8, 128], BF16)

        def run(self, t, src_d, gain_bc, gtok):
            xt = self.xt[t % 2]
            xk = ("xt", t % 2)
            B.dma("sp", xt[:], src_d[t * 128:(t + 1) * 128, :], [], [xk])
            B.act(self.hb[:], xt[:], AF.Square, [xk], ["hb", "ss"], accum=self.ss[:])
            B.act(self.rs[:], self.ss[:], AF.Sqrt, ["ss", "epsb"], ["rs"], bias=epsb[:], scale=1.0 / DM)
            B.recip(self.rs[:], self.rs[:], ["rs"], ["rs"])
            B.stt("dve", self.hb[:], xt[:], self.rs[:], gain_bc, ALU.mult, ALU.mult, [xk, "rs", gtok], ["hb"])
            for k in range(8):
                B.tr(self.pT[:, k, :], self.hb[:, k * 128:(k + 1) * 128], identb[:], ["hb", "identb"], ["pT"])
            hT = self.hT[t % 2]
            hk = ("hT", t % 2)
            B.cp("act", hT[:], self.pT[:], ["pT"], [hk])
            return xt, xk, hT, hk

    def proj(ps_ap, hT, hk, W, wk, c0, c1, pk):
        for k in range(8):
            B.mm(ps_ap, hT[:, k, :], W[:, k, c0:c1], k == 0, k == 7, [hk, wk], [pk])

    class QKNorm:
        def __init__(self, nh):
            self.nh = nh
            self.qf = B.sb("qf", [128, nh, 64])
            self.sq = B.sb("sq", [128, nh, 64])
            self.ssq = B.sb("ssq", [128, nh])
            self.qn = B.sb("qn", [128, nh, 64])
            self.tmp = B.sb("rt", [128, 4, nh, 8])

        def run(self, ps_ap, pk, t, gain_ap, gtok):
            nh = self.nh
            qf, sq, ssq, qn, tmp = self.qf, self.sq, self.ssq, self.qn, self.tmp
            B.cp("act", qf[:].rearrange("p h d -> p (h d)"), ps_ap, [pk], ["qf"])
            B.tt("dve", sq[:], qf[:], qf[:], ALU.mult, ["qf"], ["sq"])
            B.red(ssq[:], sq[:], ALU.add, ["sq"], ["ssq"])
            B.act(ssq[:], ssq[:], AF.Sqrt, ["ssq", "epsb"], ["ssq"], bias=epsb[:], scale=1.0 / 64)
            B.recip(ssq[:], ssq[:], ["ssq"], ["ssq"])
            rsb = bass.AP(ssq, 0, [[nh, 128], [1, nh], [0, 64]])
            B.tt("dve", qn[:], qf[:], rsb, ALU.mult, ["qf", "ssq"], ["qn"])
            B.tt("dve", qn[:], qn[:], gain_ap, ALU.mult, ["qn"] + (gtok if isinstance(gtok, list) else [gtok]), ["qn"])
            c = rope_ap(cosT, t, nh)
            s = rope_ap(sinT, t, nh)
            x1 = qn[:, :, 0:8]
            x2 = qn[:, :, 8:16]
            B.tt("dve", tmp[:, 0], x1, c, ALU.mult, ["qn", "cosT"], ["rt0"])
            B.tt("dve", tmp[:, 1], x2, s, ALU.mult, ["qn", "sinT"], ["rt1"])
            B.tt("dve", tmp[:, 2], x2, c, ALU.mult, ["qn", "cosT"], ["rt2"])
            B.tt("dve", tmp[:, 3], x1, s, ALU.mult, ["qn", "sinT"], ["rt3"])
            B.tt("dve", x1, tmp[:, 0], tmp[:, 1], ALU.subtract, ["rt0", "rt1"], ["qn"])
            B.tt("dve", x2, tmp[:, 2], tmp[:, 3], ALU.add, ["rt2", "rt3"], ["qn"])
            return qn

    def gain_tile(name, d, reps):
        g = B.sb(name, [128, 64])
        B.dma("sp", g[:], bcast_rows(d, 128, 64), [], [name])
        return g

    B.memset("pool", antiT[:], 0.0, ["antiT"])
    B.asel(antiT[:], antiT[:], [[-1, 128]], ALU.is_gt, NEG, 0, 1, ["antiT"], ["antiT"])
    B.stack = attn_stack
    ya = B.sb("ya", [128, NT, 512], BF16)
    B.stack = _prev_stack
    if "moba" in SKIP:
        B.memset("pool", ya[:], 0.0, [("ya", t) for t in range(NT)])
    else:
      with B.scope():
        QaT = B.sb("QaT", [128, 4, S_LEN], BF16)
        KaT = B.sb("KaT", [128, 4, S_LEN], BF16)
        Va = B.sb("Va", [128, NT, 8, 65], BF16)
        MBs = B.sb("MBs", [128, NT, 8, 16], BF16)
        kmT = B.sb("kmT", [128, 4, 16])
        B.memset("pool", kmT[:], 0.0, ["kmT"])
        B.memset("pool", Va[:, :, :, 64:65], 1.0, ["Va1"])
        IndA = B.sb("IndA", [128, 16, 128], BF16)
        B.memset("pool", IndA[:], 0.0, ["IndA"])
        B.asel(IndA[0:16], IndA[0:16], [[1, 16], [0, 128]], ALU.not_equal, 1.0, 0, -1, ["IndA"], ["IndA"])
        with B.scope():
            Wa = B.sb("Wa", [128, 8, 1536], BF16)
            for k in range(8):
                B.dma("pool", Wa[:, k, :], wa_d[k * 128:(k + 1) * 128, :], [], [("Wa", k)])
            aqg = gain_tile("aqg", aqg_d, 8)
            akg = gain_tile("akg", akg_d, 8)
            aqg_ap = bass.AP(aqg, 0, [[64, 128], [0, 8], [1, 64]])
            akg_ap = bass.AP(akg, 0, [[64, 128], [0, 8], [1, 64]])
            fr = Front()
            qkn = QKNorm(8)
            pp = [B.ps("pp", [128, 512], tok="pp%d" % i) for i in range(2)]
            ptr = B.ps("ptr", [128, 4, 128])
            pgb = [B.ps("pg", [128, 512], tok="pg%d" % i) for i in range(2)]
            pg = [pgb[i][:, 0:64].rearrange("p (h n) -> p h n", n=16) for i in range(2)]
            qTf = B.sb("qTf", [128, 4, 128])
            ksum = B.sb("ksum", [128, 4])
            gpad = B.sb("gpad", [128, 8, 16])
            m8 = B.sb("m8", [128, 8, 8])
            for t in range(NT_RUN):
                xt, xk, hT, hk = fr.run(t, x_d, n1g[:], "n1g")
                tsl = slice(t * 128, (t + 1) * 128)
                p = pp[0]
                for k in range(8):
                    B.mm(p[:], hT[:, k, :], Wa[:, k, 512:1024], k == 0, k == 7, [hk, ("Wa", k)], ["pp0"])
                kn = qkn.run(p[:], "pp0", t, akg_ap, "akg")
                for pr in range(4):
                    B.tr(ptr[:, pr, :], kn[:, 2 * pr:2 * pr + 2, :].rearrange("p h d -> p (h d)"), ident[:],
                         ["qn", "ident"], ["ptr"])
                B.cp("act", KaT[:, :, tsl], ptr[:], ["ptr"], [("KaT", t)])
                B.cp("dve", qTf[:], ptr[:], ["ptr"], ["qTf"])
                B.red(ksum[:], qTf[:], ALU.add, ["qTf"], ["ksum"])
                n = t // 2
                if t % 2 == 0:
                    B.cp("dve", kmT[:, :, n], ksum[:], ["ksum"], ["kmT"])
                else:
                    B.tt("dve", kmT[:, :, n], kmT[:, :, n], ksum[:], ALU.add, ["ksum", "kmT"], ["kmT"])
                p = pp[1]
                for k in range(8):
                    B.mm(p[:], hT[:, k, :], Wa[:, k, 0:512], k == 0, k == 7, [hk, ("Wa", k)], ["pp1"])
                qn = qkn.run(p[:], "pp1", t, aqg_ap, "aqg")
                for pr in range(4):
                    B.tr(ptr[:, pr, :], qn[:, 2 * pr:2 * pr + 2, :].rearrange("p h d -> p (h d)"), ident[:],
                         ["qn", "ident"], ["ptr"])
                B.cp("act", QaT[:, :, tsl], ptr[:], ["ptr"], [("QaT", t)])
                B.cp("dve", qTf[:], ptr[:], ["ptr"], ["qTf"])
                for h in range(8):
                    po = (h % 2) * 64
                    B.mm(pg[h % 2][:, h // 2, :], qTf[po:po + 64, h // 2, :], kmT[po:po + 64, h // 2, :], True, True,
                         ["qTf", "kmT"], ["pg%d" % (h % 2)])
                own = t // 2
                if own > 0:
                    gp4 = gpad[:].rearrange("p (a b) n -> p a b n", b=2)
                    B.cp("dve", gp4[:, :, 0, :], pg[0], ["pg0"], ["gpad"])
                    B.cp("dve", gp4[:, :, 1, :], pg[1], ["pg1"], ["gpad"])
                    B.memset("dve", gpad[:, :, own:16], -BIGF, ["gpad"])
                    for h in range(8):
                        B.max8(m8[:, h, :], gpad[:, h, :], ["gpad"], ["m8"])
                    thr = bass.AP(m8, 2, [[64, 128], [8, 8], [0, 16]])
                    B.tt("dve", gpad[:], gpad[:], thr, ALU.is_lt, ["gpad", "m8"], ["gpad"])
                    B.ts("dve", MBs[:, t], gpad[:], NEG, None, ALU.mult, None, ["gpad"], [("MBs", t)])
                p = pp[0]
                for k in range(8):
                    B.mm(p[:], hT[:, k, :], Wa[:, k, 1024:1536], k == 0, k == 7, [hk, ("Wa", k)], ["pp0"])
                B.cp("act", Va[:, t, :, 0:64], p[:].rearrange("p (h d) -> p h d", d=64), ["pp0"], [("Va", t)])
            B.dump("QaT", QaT[:, 0, 0:NT_RUN * 128], [("QaT", t) for t in range(NT_RUN)], [128, NT_RUN * 128], BF16)
            B.dump("KaT", KaT[:, 3, 0:NT_RUN * 128], [("KaT", t) for t in range(NT_RUN)], [128, NT_RUN * 128], BF16)
            B.dump("MBs", MBs[:, 2:NT_RUN].rearrange("p t h n -> p (t h n)"), [("MBs", t) for t in range(2, NT_RUN)], [128, (NT_RUN - 2) * 128], BF16)

        if stop_after == "A1":
            pass
        else:
            with B.scope():
                NS = 3
                sps = [B.ps("sps", [128, 4, 128], tok=("sps", i)) for i in range(NS)]
                ops_ = [B.ps("ops", [128, 512], tok=("ops", i)) for i in range(2)]
                pmb = B.ps("pmb", [16, 8, 128], BF16)
                PT = [B.sb("PT", [128, 4, 128], BF16) for _ in range(NS)]
                MBT = [B.sb("MBT", [128, 8, 128], BF16) for _ in range(2)]
                for i_ in range(2):
                    B.memset("pool", MBT[i_][:], 0.0, [("MBT", i_)])
                rinv = [B.sb("rinv", [128, 1]) for _ in range(2)]
                gi = 0
                oi = 0
                for qi in range(NT_RUN):
                    qsl = slice(qi * 128, (qi + 1) * 128)
                    own = qi // 2
                    mk = ("MBT", qi % 2)
                    if own > 0:
                        for h in range(8):
                            B.tr(pmb[:, h, :], MBs[:, qi, h, :], identb[:], [("MBs", qi), "identb"], ["pmb"])
                        B.cp("act", MBT[qi % 2][0:16], pmb[:], ["pmb"], [mk])
                    for h in range(8):
                        po = (h % 2) * 64
                        pr = h // 2
                        qT = QaT[po:po + 64, pr, qsl]
                        o = ops_[oi % 2]
                        ok = ("ops", oi % 2)
                        oi += 1
                        kts = list(range(qi + 1))
                        ngrp = (len(kts) + 3) // 4

                        def issue_qk(g):
                            nonlocal gi
                            grp = kts[4 * g:4 * g + 4]
                            sp = sps[gi % NS]
                            sk = ("sps", gi % NS)
                            pt = PT[gi % NS]
                            pk = ("PT", gi % NS)
                            gi += 1
                            for j, kt in enumerate(grp):
                                ksl = slice(kt * 128, (kt + 1) * 128)
                                past = (kt // 2) < own
                                diag = kt == qi
                                B.mm(sp[:, j, :], KaT[po:po + 64, pr, ksl], qT, True, not (past or diag),
                                     [("KaT", kt), ("QaT", qi)], [sk])
                                if past:
                                    B.mm(sp[:, j, :], IndA[:, kt // 2, :], MBT[qi % 2][:, h, :], False, True, ["IndA", mk], [sk])
                                if diag:
                                    B.mm(sp[:, j, :], identb[:], causT[:], False, True, ["identb", "causT"], [sk])
                            return grp, sp, sk, pt, pk

                        cur = issue_qk(0)
                        for g in range(ngrp):
                            nxt = issue_qk(g + 1) if g + 1 < ngrp else None
                            grp, sp, sk, pt, pk = cur
                            nj = len(grp)
                            B.act(pt[:, 0:nj, :], sp[:, 0:nj, :], AF.Exp, [sk], [pk], scale=SCALE)
                            for j, kt in enumerate(grp):
                                first = (g == 0 and j == 0)
                                last = (g == ngrp - 1 and j == nj - 1)
                                B.mm(o[:, 0:65], pt[:, j, :], Va[:, kt, h, :], first, last, [pk, ("Va", kt), "Va1"], [ok])
                            cur = nxt
                        ri = rinv[oi % 2]
                        rk = ("rinv", oi % 2)
                        B.recip(ri[:], o[:, 64:65], [ok], [rk])
                        B.ts("dve", ya[:, qi, h * 64:(h + 1) * 64], o[:, 0:64], ri[:], None, ALU.mult, None, [ok, rk], [("ya", qi)])
            B.dump("ya", ya[:, 0:NT_RUN].rearrange("p t c -> p (t c)"), [("ya", t) for t in range(NT_RUN)], [128, NT_RUN * 512], BF16)


    stop("B1")
    B.stack = attn_stack
    yb = B.sb("yb", [128, NT, 512], BF16)
    B.stack = _prev_stack
    with B.scope():
        QbT = B.sb("QbT", [128, 4, S_LEN], BF16)
        KsT = B.sb("KsT", [128, S_LEN], BF16)
        KwT = B.sb("KwT", [128, S_LEN], BF16)
        Vs = B.sb("Vs", [128, NT, 2, 65], BF16)
        Vw = B.sb("Vw", [128, NT, 2, 65], BF16)
        KcT = B.sb("KcT", [128, 256], BF16)
        Vc = B.sb("Vc", [128, 2, 2, 129], BF16)
        gbt = B.sb("gbt", [128, NT, 24])
        B.memset("pool", Vs[:, :, :, 64:65], 1.0, ["Vs1"])
        B.memset("pool", Vw[:, :, :, 64:65], 1.0, ["Vw1"])
        with B.scope():
            bkcT = B.sb("bkcT", [128, 16, 256], BF16)
            bvcT = B.sb("bvcT", [128, 16, 256], BF16)
            with B.scope():
                Wb = B.sb("Wb", [128, 8, 1304], BF16)
                for k in range(8):
                    B.dma("pool", Wb[:, k, :], wb_d[k * 128:(k + 1) * 128, :], [], [("Wb", k)])
                bqg = gain_tile("bqg", bqg_d, 8)
                bqg_ap = bass.AP(bqg, 0, [[64, 128], [0, 8], [1, 64]])
                g4 = B.sb("g4", [128, 4, 64])
                B.dma("sp", g4[:, 0, :], bcast_rows(bksg_d, 128, 64), [], ["g4a"])
                B.dma("sp", g4[:, 1, :], bcast_rows(bksg_d, 128, 64), [], ["g4b"])
                B.dma("sp", g4[:, 2, :], bcast_rows(bkwg_d, 128, 64), [], ["g4c"])
                B.dma("sp", g4[:, 3, :], bcast_rows(bkwg_d, 128, 64), [], ["g4d"])
                g4toks = ["g4a", "g4b", "g4c", "g4d"]
                fr = Front()
                qkn8 = QKNorm(8)
                qkn4 = QKNorm(4)
                pp = [B.ps("pp", [128, 512], tok="pp%d" % i) for i in range(2)]
                ptr = B.ps("ptr", [128, 4, 128])
                raw = B.sb("raw", [128, 256])
                for t in range(NT_RUN):
                    xt, xk, hT, hk = fr.run(t, x_d, n1g[:], "n1g")
                    tsl = slice(t * 128, (t + 1) * 128)
                    p = pp[0]
                    for k in range(8):
                        B.mm(p[:], hT[:, k, :], Wb[:, k, 0:512], k == 0, k == 7, [hk, ("Wb", k)], ["pp0"])
                    qn = qkn8.run(p[:], "pp0", t, bqg_ap, "bqg")
                    for r in range(4):
                        B.tr(ptr[:, r, :], qn[:, 2 * r:2 * r + 2, :].rearrange("p h d -> p (h d)"), ident[:],
                             ["qn", "ident"], ["ptr"])
                    B.cp("act", QbT[:, :, tsl], ptr[:], ["ptr"], [("QbT", t)])
                    p = pp[1]
                    for k in range(8):
                        B.mm(p[:], hT[:, k, :], Wb[:, k, 512:1024], k == 0, k == 7, [hk, ("Wb", k)], ["pp1"])
                    B.cp("act", raw[:], p[:, 256:512], ["pp1"], ["raw"])
                    kn = qkn4.run(p[:, 0:256], "pp1", t, g4[:], g4toks)
                    B.tr(ptr[:, 0, :], kn[:, 0:2, :].rearrange("p h d -> p (h d)"), ident[:], ["qn", "ident"], ["ptr"])
                    B.tr(ptr[:, 1, :], kn[:, 2:4, :].rearrange("p h d -> p (h d)"), ident[:], ["qn", "ident"], ["ptr"])
                    B.tr(ptr[:, 2, :], raw[:, 0:128], ident[:], ["raw", "ident"], ["ptr"])
                    B.tr(ptr[:, 3, :], raw[:, 128:256], ident[:], ["raw", "ident"], ["ptr"])
                    B.cp("act", KsT[:, tsl], ptr[:, 0, :], ["ptr"], [("KsT", t)])
                    B.cp("act", KwT[:, tsl], ptr[:, 1, :], ["ptr"], [("KwT", t)])
                    B.cp("act", bkcT[:, :, 8 * t:8 * t + 8], ptr[:, 2, :].rearrange("p (n f) -> p f n", f=16), ["ptr"], [("bkcT", t)])
                    B.cp("act", bvcT[:, :, 8 * t:8 * t + 8], ptr[:, 3, :].rearrange("p (n f) -> p f n", f=16), ["ptr"], [("bvcT", t)])
                    p = pp[0]
                    for k in range(8):
                        B.mm(p[:, 0:280], hT[:, k, :], Wb[:, k, 1024:1304], k == 0, k == 7, [hk, ("Wb", k)], ["pp0"])
                    B.cp("act", Vs[:, t, :, 0:64], p[:, 0:128].rearrange("p (g d) -> p g d", d=64), ["pp0"], [("Vs", t)])
                    B.cp("act", Vw[:, t, :, 0:64], p[:, 128:256].rearrange("p (g d) -> p g d", d=64), ["pp0"], [("Vw", t)])
                    B.act(gbt[:, t, :], p[:, 256:280], AF.Sigmoid, ["pp0"], [("gbt", t)])
            stop("A2")
            with B.scope():
                ovl = B.sb("ovl", [128, 2, 64])
                B.memset("pool", ovl[:], 1.0, ["ovl"])
                for nt_ in range(2):
                    B.asel(ovl[:, nt_, :], ovl[:, nt_, :], [[-4, 64]], ALU.is_ge, 0.0, 128 * nt_ + 1, 1, ["ovl"], ["ovl"])
                    B.asel(ovl[:, nt_, :], ovl[:, nt_, :], [[4, 64]], ALU.is_ge, 0.0, 3 - 128 * nt_, -1, ["ovl"], ["ovl"])
                for nt_ in range(2):
                    for g in range(2):
                        B.cp("dve", Vc[:, nt_, g, 65:129], ovl[:, nt_, :], ["ovl"], [("Vco", nt_, g)])
                B.memset("pool", Vc[:, :, :, 64:65], 1.0, ["Vc1"])
                bkcg = gain_tile("bkcg", bkcg_d, 1)
                ph = [B.ps("ph", [128, 512], tok="ph%d" % i) for i in range(2)]
                po2 = B.ps("po2", [128, 512])
                ptr2 = B.ps("ptr2", [128, 4, 128])
                kcn = B.sb("kcn", [128, 2, 2, 64])
                for kv in ("k", "v"):
                    if kv == "v" and stop_after == "CMPe":
                        B.dump("kcn", kcn[:].rearrange("p a b c -> p (a b c)"), [("kcn", 0), ("kcn", 1)], [128, 256])
                        stop("CMPe")
                    cd = cmp_d[kv]
                    srcT = bkcT if kv == "k" else bvcT
                    stoks = [(("bkcT" if kv == "k" else "bvcT"), t) for t in range(NT_RUN)]
                    w1s = B.sb("w1s", [128, 32, 256], BF16)
                    w1v = cd["w1"].ap().rearrange("(l d) c -> d l c", d=64)
                    B.dma("pool", w1s[0:64], w1v, [], [("w1s", kv, 0)])
                    B.dma("pool", w1s[64:128], w1v, [], [("w1s", kv, 1)])
                    posT = B.sb("posT", [128, 32, 2], BF16)
                    B.dma("pool", posT[0:64].rearrange("p l c -> p (l c)"), cd["posT"].ap(), [], [("posT", kv)])
                    B.dma("pool", posT[64:128].rearrange("p l c -> p (l c)"), cd["posT"].ap(), [], [("posT2", kv)])
                    b1t = B.sb("b1t", [128, 2])
                    B.dma("sp", b1t[:], cd["b1"].ap(), [], [("b1t", kv)])
                    w2s = B.sb("w2s", [128, 2, 64], BF16)
                    B.dma("pool", w2s[:], cd["w2"].ap().rearrange("(k p) d -> p k d", p=128), [], [("w2s", kv)])
                    b2t = B.sb("b2t", [128, 64])
                    B.dma("sp", b2t[:], bcast_rows(cd["b2"], 128, 64), [], [("b2t", kv)])
                    if stop_after == "CMPa":
                        B.dump("w1s", w1s[:, 5, :], [("w1s", kv, 0), ("w1s", kv, 1)], [128, 256], BF16)
                        B.dump("w2s", w2s[:].rearrange("p a b -> p (a b)"), [("w2s", kv)], [128, 128], BF16)
                        B.dump("b1t", b1t[:], [("b1t", kv)], [128, 2])
                        B.dump("posT", posT[:].rearrange("p a b -> p (a b)"), [("posT", kv), ("posT2", kv)], [128, 64], BF16)
                        stop("CMPa")
                    hidT = B.sb("hidT", [128, 2, 256], BF16)
                    B.memset("pool", hidT[:, :, 255:256], 0.0, [("hidT", 0), ("hidT", 1)])
                    biasc = B.sb("biasc", [128, 1])
                    cf = B.sb("cf", [128, 2, 64])
                    sq2 = B.sb("sq2", [128, 2, 64])
                    ss2 = B.sb("ss2", [128, 2])
                    for g in range(2):
                        for cc in range(2):
                            phb = ph[cc]
                            pk_ = "ph%d" % cc
                            csl = slice(cc * 128, (cc + 1) * 128)
                            for l in range(32):
                                B.mm(phb[:, 256:258], w1s[g * 64:(g + 1) * 64, l, csl], posT[g * 64:(g + 1) * 64, l, :], l == 0, l == 31,
                                     [("w1s", kv, g), ("posT", kv), ("posT2", kv)], [pk_])
                            for l in range(32):
                                rhs = srcT[g * 64:(g + 1) * 64, l % 16, l // 16:l // 16 + 255]
                                B.mm(phb[:, 0:255], w1s[g * 64:(g + 1) * 64, l, csl], rhs, l == 0, l == 31,
                                     [("w1s", kv, g)] + stoks, [pk_])
                            B.tt("dve", biasc[:], phb[:, 256:257], b1t[:, cc:cc + 1], ALU.add, [pk_, ("b1t", kv)], ["biasc"])
                            if stop_after == "CMPb" or (stop_after == "CMPg1" and g == 1):
                                B.dump("biasc", biasc[:], ["biasc"], [128, 1])
                                stop(stop_after)
                            B.act(hidT[:, cc, 0:255], phb[:, 0:255], AF.Gelu_apprx_tanh, [pk_, "biasc"], [("hidT", cc)], bias=biasc[:])
                        if stop_after == "CMPc":
                            B.dump("hidT", hidT[:].rearrange("p a b -> p (a b)"), [("hidT", 0), ("hidT", 1)], [128, 512], BF16)
                            stop("CMPc")
                        for nt_ in range(2):
                            for cc in range(2):
                                B.mm(po2[:, nt_ * 64:(nt_ + 1) * 64], hidT[:, cc, nt_ * 128:(nt_ + 1) * 128], w2s[:, cc, :],
                                     cc == 0, cc == 1, [("hidT", cc), ("w2s", kv)], ["po2"])
                        b2b = bass.AP(b2t, 0, [[64, 128], [0, 2], [1, 64]])
                        B.tt("dve", cf[:], po2[:, 0:128].rearrange("p (n d) -> p n d", d=64), b2b, ALU.add, ["po2", ("b2t", kv)], ["cf"])
                        if stop_after == "CMPd":
                            B.dump("cf", cf[:].rearrange("p a b -> p (a b)"), ["cf"], [128, 128])
                            stop("CMPd")
                        if kv == "v":
                            B.cp("act", Vc[:, :, g, 0:64], cf[:], ["cf"], [("Vcv", g)])
                        else:
                            B.tt("dve", sq2[:], cf[:], cf[:], ALU.mult, ["cf"], ["sq2"])
                            B.red(ss2[:], sq2[:], ALU.add, ["sq2"], ["ss2"])
                            B.act(ss2[:], ss2[:], AF.Sqrt, ["ss2", "epsb"], ["ss2"], bias=epsb[:], scale=1.0 / 64)
                            B.recip(ss2[:], ss2[:], ["ss2"], ["ss2"])
                            B.tt("dve", cf[:], cf[:], bass.AP(ss2, 0, [[2, 128], [1, 2], [0, 64]]), ALU.mult, ["cf", "ss2"], ["cf"])
                            B.tt("dve", kcn[:, :, g, :], cf[:], bass.AP(bkcg, 0, [[64, 128], [0, 2], [1, 64]]), ALU.mult,
                                 ["cf", "bkcg"], [("kcn", g)])
                            if stop_after == "CMPd2":
                                B.dump("kcn", kcn[:, :, 0, :], [("kcn", 0)], [128, 2, 64])
                                stop("CMPd2")
                if stop_after == "CMPf":
                    B.dump("kcn", kcn[:].rearrange("p a b c -> p (a b c)"), [("kcn", 0), ("kcn", 1)], [128, 256])
                    stop("CMPf")
                for nt_ in range(2):
                    B.tr(ptr2[:, nt_, :], kcn[:, nt_, :, :].rearrange("p g d -> p (g d)"), ident[:],
                         [("kcn", 0), ("kcn", 1), "ident"], ["ptr2"])
                B.cp("act", KcT[:].rearrange("p (n a) -> p n a", a=128), ptr2[:, 0:2, :], ["ptr2"], ["KcT"])
                B.dump("KcT", KcT[:], "KcT", [128, 256], BF16)
                B.dump("Vc", Vc[:].rearrange("p a b c -> p (a b c)"), [("Vcv", 0), ("Vcv", 1), "Vc1", ("Vco", 0, 0), ("Vco", 0, 1), ("Vco", 1, 0), ("Vco", 1, 1)], [128, 516], BF16)
        B.dump("QbT", QbT[:, 1, 0:NT_RUN * 128], [("QbT", t) for t in range(NT_RUN)], [128, NT_RUN * 128], BF16)
        B.dump("KsT", KsT[:, 0:NT_RUN * 128], [("KsT", t) for t in range(NT_RUN)], [128, NT_RUN * 128], BF16)
        B.dump("KwT", KwT[:, 0:NT_RUN * 128], [("KwT", t) for t in range(NT_RUN)], [128, NT_RUN * 128], BF16)
        B.dump("gbt", gbt[:, 0:NT_RUN].rearrange("p t c -> p (t c)"), [("gbt", t) for t in range(NT_RUN)], [128, NT_RUN * 24])
        stop("CMP")
        with B.scope():
            NS = 3
            sps = [B.ps("sps", [128, 4, 128], tok=("sps", i)) for i in range(NS)]
            ops_ = [B.ps("ops", [128, 512], tok=("ops", i)) for i in range(2)]
            pmb = B.ps("pmb", [64, 8, 128], BF16)
            PT = [B.sb("PT", [128, 4, 128], BF16) for _ in range(NS)]
            IndS = B.sb("IndS", [128, NT, 128], BF16)
            B.memset("pool", IndS[:], 0.0, ["IndS"])
            for hf in range(2):
                B.asel(IndS[0:64, :, hf * 64:(hf + 1) * 64], IndS[0:64, :, hf * 64:(hf + 1) * 64], [[2, NT], [0, 64]],
                       ALU.not_equal, 1.0, hf, -1, ["IndS"], ["IndS"])
            Ttab = B.sb("Ttab", [128, 128])
            B.memset("pool", Ttab[:], 0.0, ["Ttab"])
            for hf in range(2):
                rows = slice(64 * hf, 64 * hf + 64)
                B.asel(Ttab[rows, :], Ttab[rows, :], [[-1, 128]], ALU.is_ge, -BIGF, 64 + hf, 0, ["Ttab"], ["Ttab"])
                B.asel(Ttab[rows, :], Ttab[rows, :], [[1, 128]], ALU.not_equal, BIGF, -(64 + hf), 0, ["Ttab"], ["Ttab"])
                B.asel(Ttab[rows, :], Ttab[rows, :], [[1, 128]], ALU.not_equal, BIGF, -(63 + hf), 0, ["Ttab"], ["Ttab"])
            tiny = B.sb("tiny", [128, 1])
            B.memset("dve", tiny[:], 1e-30, ["tiny"])
            cmk = [B.sb("cmk", [128, 128], BF16) for _ in range(4)]
            ybf = B.sb("ybf", [128, 512])
            impg = [B.sb("impg", [128, 64]) for _ in range(2)]
            imp2 = B.sb("imp2", [128, 64])
            m8a = B.sb("m8a", [128, 8])
            m8b = B.sb("m8b", [128, 8])
            MBg = B.sb("MBg", [128, 64], BF16)
            MBTs = [[B.sb("MBTs", [128, 128], BF16) for _ in range(2)] for _ in range(2)]
            for g_ in range(2):
                for i_ in range(2):
                    B.memset("pool", MBTs[g_][i_][:], 0.0, [("MBTs", g_, i_)])
            rv = [B.sb("rv", [128, 1]) for _ in range(4)]
            cf_ = [B.sb("cfc", [128, 1]) for _ in range(4)]

            class St:
                gi = 0
                oi = 0
                ci = 0
                ri = 0

            def attend(qT, qtok, tiles, ncols):
                o = ops_[St.oi % 2]
                ok = ("ops", St.oi % 2)
                St.oi += 1
                ngrp = (len(tiles) + 3) // 4

                def issue_qk(g_):
                    grp = tiles[4 * g_:4 * g_ + 4]
                    sp = sps[St.gi % NS]
                    sk = ("sps", St.gi % NS)
                    pt = PT[St.gi % NS]
                    pk = ("PT", St.gi % NS)
                    St.gi += 1
                    for j, (kT, ktok, V, vtoks, masks) in enumerate(grp):
                        B.mm(sp[:, j, :], kT, qT, True, len(masks) == 0, [ktok, qtok], [sk])
                        for mi, (ml, mr, mt) in enumerate(masks):
                            B.mm(sp[:, j, :], ml, mr, False, mi == len(masks) - 1, mt, [sk])
                    return grp, sp, sk, pt, pk

                cur = issue_qk(0)
                for g_ in range(ngrp):
                    nxt = issue_qk(g_ + 1) if g_ + 1 < ngrp else None
                    grp, sp, sk, pt, pk = cur
                    nj = len(grp)
                    B.act(pt[:, 0:nj, :], sp[:, 0:nj, :], AF.Exp, [sk], [pk], scale=SCALE)
                    for j, (kT, ktok, V, vtoks, masks) in enumerate(grp):
                        B.mm(o[:, 0:ncols], pt[:, j, :], V, g_ == 0 and j == 0, g_ == ngrp - 1 and j == nj - 1,
                             [pk] + vtoks, [ok])
                    cur = nxt
                return o, ok

            def coef(o, ok, gcol_ap, gtok):
                r_ = rv[St.ri % 4]
                rk = ("rv", St.ri % 4)
                c_ = cf_[St.ri % 4]
                ck = ("cfc", St.ri % 4)
                St.ri += 1
                B.tt("dve", r_[:], o[:, 64:65], tiny[:], ALU.max, [ok, "tiny"], [rk])
                B.recip(r_[:], r_[:], [rk], [rk])
                B.tt("dve", c_[:], r_[:], gcol_ap, ALU.mult, [rk, gtok], [ck])
                return r_, rk, c_, ck

            qlist = list(range(NT_RUN)) if QT_LIST is None else list(QT_LIST)
            for qi in qlist:
                qsl = slice(qi * 128, (qi + 1) * 128)
                qtok = ("QbT", qi)
                ctiles = []
                for kt in range(2):
                    base = 128 * qi - 2048 * kt - 31
                    if base + 127 < 0:
                        continue
                    masks = []
                    if base - 16 * 127 < 0:
                        cm_ = cmk[St.ci % 4]
                        ck_ = ("cmk", St.ci % 4)
                        St.ci += 1
                        B.memset("pool", cm_[:], 0.0, [ck_])
                        B.asel(cm_[:], cm_[:], [[1, 128]], ALU.is_ge, NEG, base, -16, [ck_], [ck_])
                        masks = [(identb[:], cm_[:], ["identb", ck_])]
                    ctiles.append((kt, masks))
                for g in range(2):
                    gs = slice(g * 64, (g + 1) * 64)
                    for r in range(4):
                        h = g * 4 + r
                        hs = slice(h * 64, (h + 1) * 64)
                        qT = QbT[gs, r, qsl]
                        if ctiles:
                            tiles = [(KcT[gs, kt * 128:(kt + 1) * 128], "KcT", Vc[:, kt, g, :],
                                      [("Vcv", g), "Vc1", ("Vco", kt, g)], masks) for kt, masks in ctiles]
                            o, ok = attend(qT, qtok, tiles, 129)
                            r_, rk, c_, ck = coef(o, ok, gbt[:, qi, h * 3:h * 3 + 1], ("gbt", qi))
                            B.ts("dve", ybf[:, hs], o[:, 0:64], c_[:], None, ALU.mult, None, [ok, ck], [("ybf", h)])
                            if r == 0:
                                B.ts("dve", impg[g][:], o[:, 65:129], r_[:], None, ALU.mult, None, [ok, rk], [("impg", g)])
                            else:
                                B.stt("dve", impg[g][:], o[:, 65:129], r_[:], impg[g][:], ALU.mult, ALU.add,
                                      [ok, rk, ("impg", g)], [("impg", g)])
                        else:
                            B.memset("dve", ybf[:, hs], 0.0, [("ybf", h)])
                            if r == 0:
                                B.memset("dve", impg[g][:], 0.0, [("impg", g)])
                    ik = ("impg", g)
                    B.tt("dve", impg[g][:], impg[g][:], Ttab[:, 64 - 2 * qi:128 - 2 * qi], ALU.add, [ik, "Ttab"], [ik])
                    B.memset("dve", impg[g][:, 0:1], BIGF, [ik])
                    B.max8(m8a[:], impg[g][:], [ik], ["m8a"])
                    B.mrep(imp2[:], m8a[:], impg[g][:], -3.0e38, [ik, "m8a"], ["imp2"])
                    B.max8(m8b[:], imp2[:], ["imp2"], ["m8b"])
                    B.ts("dve", MBg[:], impg[g][:], m8b[:, 7:8], NEG, ALU.is_lt, ALU.mult, [ik, "m8b"], ["MBg"])
                    B.tr(pmb[:, 0, :], MBg[:], identb[:], ["MBg", "identb"], ["pmb"])
                    mbt = MBTs[g][qi % 2]
                    mk = ("MBTs", g, qi % 2)
                    B.cp("act", mbt[0:64], pmb[:, 0, :], ["pmb"], [mk])
                    if qi == qlist[-1] and g == 0:
                        B.dump("MBg", MBg[:], "MBg", [128, 64], BF16)
                    for r in range(4):
                        h = g * 4 + r
                        hs = slice(h * 64, (h + 1) * 64)
                        qT = QbT[gs, r, qsl]
                        tiles = []
                        for kt in range(qi + 1):
                            ksl = slice(kt * 128, (kt + 1) * 128)
                            if kt == qi:
                                masks = [(identb[:], causT[:], ["identb", "causT"])]
                            else:
                                masks = [(IndS[:, kt, :], mbt[:], ["IndS", mk])]
                            tiles.append((KsT[gs, ksl], ("KsT", kt), Vs[:, kt, g, :], [("Vs", kt), "Vs1"], masks))
                        o, ok = attend(qT, qtok, tiles, 65)
                        r_, rk, c_, ck = coef(o, ok, gbt[:, qi, h * 3 + 1:h * 3 + 2], ("gbt", qi))
                        B.stt("dve", ybf[:, hs], o[:, 0:64], c_[:], ybf[:, hs], ALU.mult, ALU.add, [ok, ck, ("ybf", h)], [("ybf", h)])
                        tiles = []
                        for kt in range(max(0, qi - 4), qi + 1):
                            ksl = slice(kt * 128, (kt + 1) * 128)
                            masks = []
                            if kt == qi:
                                masks = [(identb[:], causT[:], ["identb", "causT"])]
                            elif kt == qi - 4:
                                masks = [(identb[:], antiT[:], ["identb", "antiT"])]
                            tiles.append((KwT[gs, ksl], ("KwT", kt), Vw[:, kt, g, :], [("Vw", kt), "Vw1"], masks))
                        o, ok = attend(qT, qtok, tiles, 65)
                        r_, rk, c_, ck = coef(o, ok, gbt[:, qi, h * 3 + 2:h * 3 + 3], ("gbt", qi))
                        B.stt("dve", ybf[:, hs], o[:, 0:64], c_[:], ybf[:, hs], ALU.mult, ALU.add, [ok, ck, ("ybf", h)], [("ybf", h)])
                B.cp("act", yb[:, qi, :], ybf[:], [("ybf", h) for h in range(8)], [("yb", qi)])
    if QT_LIST is None:
        B.dump("yb", yb[:, 0:NT_RUN].rearrange("p t c -> p (t c)"), [("yb", t) for t in range(NT_RUN)], [128, NT_RUN * 512], BF16)
    else:
        for qi in QT_LIST:
            B.dump("yb%d" % qi, yb[:, qi, :], [("yb", qi)], [128, 512], BF16)
    stop("B2")


    with B.scope():
        Wm = B.sb("Wm", [128, 8, 2048], BF16)
        for k in range(8):
            B.dma("pool", Wm[:, k, :], wm_d[k * 128:(k + 1) * 128, :], [], [("Wm", k)])
        wua = B.sb("wua", [128, 4, DM], BF16)
        wub = B.sb("wub", [128, 4, DM], BF16)
        for k in range(4):
            B.dma("pool", wua[:, k, :], wua_d[k * 128:(k + 1) * 128, :], [], [("wua", k)])
            B.dma("pool", wub[:, k, :], wub_d[k * 128:(k + 1) * 128, :], [], [("wub", k)])
        wo = B.sb("wo", [128, 8, DM], BF16)
        for k in range(8):
            B.dma("pool", wo[:, k, :], wo_d[k * 128:(k + 1) * 128, :], [], [("wo", k)])
        bmb = B.sb("bmb", [128, 2048])
        B.dma("sp", bmb[:], bcast_rows(bm_d, 128, 2048), [], ["bmb"])
        fr = Front()
        pp = [B.ps("pp", [128, 512], tok="pp%d" % i) for i in range(2)]
        pa = [B.ps("pa", [128, 512], tok="pa%d" % i) for i in range(2)]
        gt = B.sb("gt", [128, 2048], BF16)
        tmpg = B.sb("tmpg", [128, 512])
        yT = B.sb("yT", [128, 8, 128], BF16)
        m1 = B.sb("m1", [128, DM])
        mg = B.sb("mg", [128, DM], BF16)
        mT = B.sb("mT", [128, 8, 128], BF16)
        x2t = [B.sb("x2t", [128, DM]) for _ in range(2)]
        for t in range(NT_RUN):
            xt, xk, hT, hk = fr.run(t, x_d, n1g[:], "n1g")
            for cg in range(4):
                p = pp[cg % 2]
                pk = "pp%d" % (cg % 2)
                cs = slice(cg * 512, (cg + 1) * 512)
                for k in range(8):
                    B.mm(p[:], hT[:, k, :], Wm[:, k, cs], k == 0, k == 7, [hk, ("Wm", k)], [pk])
                B.tt("dve", tmpg[:], p[:], bmb[:, cs], ALU.add, [pk, "bmb"], ["tmpg"])
                B.act(gt[:, cs], tmpg[:], AF.Sigmoid, ["tmpg"], [("gt", cg)])
            for k in range(4):
                B.tr(fr.pT[:, k, :], ya[:, t, k * 128:(k + 1) * 128], identb[:], [("ya", t), "identb"], ["pT"])
            for k in range(4):
                B.tr(fr.pT[:, 4 + k, :], yb[:, t, k * 128:(k + 1) * 128], identb[:], [("yb", t), "identb"], ["pT"])
            B.cp("act", yT[:], fr.pT[:], ["pT"], ["yT"])
            for half in range(2):
                hs_ = slice(half * 512, (half + 1) * 512)
                pk = "pa%d" % half
                for k in range(4):
                    B.mm(pa[half][:], yT[:, k, :], wua[:, k, hs_], k == 0, k == 3, ["yT", ("wua", k)], [pk])
                B.tt("dve", m1[:, hs_], pa[half][:], gt[:, hs_], ALU.mult, [pk, ("gt", half)], [("m1", half)])
            for half in range(2):
                hs_ = slice(half * 512, (half + 1) * 512)
                pk = "pa%d" % half
                for k in range(4):
                    B.mm(pa[half][:], yT[:, 4 + k, :], wub[:, k, hs_], k == 0, k == 3, ["yT", ("wub", k)], [pk])
                B.tt("dve", tmpg[:], pa[half][:], gt[:, 1024 + half * 512:1024 + (half + 1) * 512], ALU.mult,
                     [pk, ("gt", 2 + half)], ["tmpg"])
                B.tt("dve", mg[:, hs_], tmpg[:], m1[:, hs_], ALU.add, ["tmpg", ("m1", half)], [("mg", half)])
            for k in range(8):
                B.tr(fr.pT[:, k, :], mg[:, k * 128:(k + 1) * 128], identb[:], [("mg", k // 4), "identb"], ["pT"])
            B.cp("act", mT[:], fr.pT[:], ["pT"], ["mT"])
            x2 = x2t[t % 2]
            for half in range(2):
                hs_ = slice(half * 512, (half + 1) * 512)
                pk = "pa%d" % half
                for k in range(8):
                    B.mm(pa[half][:], mT[:, k, :], wo[:, k, hs_], k == 0, k == 7, ["mT", ("wo", k)], [pk])
                B.tt("dve", x2[:, hs_], pa[half][:], xt[:, hs_], ALU.add, [pk, xk], [("x2t", t % 2, half)])
            B.dma("sp", x2_d[t * 128:(t + 1) * 128, :], x2[:], [("x2t", t % 2, 0), ("x2t", t % 2, 1)], [("x2d", t)])
    B.S.barrier()
    attn_stack.close()
    B.dump("x2", x2_d[0:NT_RUN * 128, :], [("x2d", t) for t in range(NT_RUN)], [NT_RUN * 128, DM])
    stop("M")

    with B.scope():
        n2g = B.sb("n2g", [128, DM])
        B.dma("sp", n2g[:], bcast_rows(n2g_d, 128, DM), [], ["n2g"])
        wq = B.sb("wq", [128, 8, 2048], BF16)
        for k in range(8):
            B.dma("pool", wq[:, k, :], wq_d[k * 128:(k + 1) * 128, :], [], [("wq", k)])
        kT = [B.sb("k1T", [128, 128], BF16), B.sb("k2T", [128, 128], BF16)]
        B.dma("pool", kT[0][:], k1T_d.ap(), [], ["k1T"])
        B.dma("pool", kT[1][:], k2T_d.ap(), [], ["k2T"])
        ktok = ["k1T", "k2T"]
        h2Tg = B.sb("h2Tg", [128, 8, 512], BF16)
        eaz = B.sb("eaz", [128, 4, 8, 128])
        ebz = B.sb("ebz", [128, 4, 8, 128])
        thp = B.sb("thp", [128, 4, 8])
        WTs = [B.sb("WT", [128, 16, 512], BF16) for _ in range(2)]
        oaccT = B.sb("oaccT", [128, 8, 512])
        pT = B.ps("pT", [128, 8, 128], BF16)
        pq = [B.ps("pq", [128, 512], tok="pq%d" % i) for i in range(2)]
        acc = [B.ps("acc", [128, 512], tok="acc%d" % i) for i in range(4)]
        uTv = uT_d.ap().rearrange("(k p) e -> p k e", p=128)
        NG = max(1, NT_RUN // 4) if PEER_GROUPS is None else PEER_GROUPS
        for G in range(NG):
            h2toks = [("h2Tg", tt) for tt in range(4)]
            with B.scope():
                hb2 = B.sb("hb2", [128, DM], BF16)
                x2r = [B.sb("x2r", [128, DM]) for _ in range(2)]
                ss = B.sb("ss", [128, 1])
                rs = B.sb("rs", [128, 1])
                qTs = B.sb("qTs", [128, 16, 512], BF16)
                s12 = [B.sb("s1", [128, 8, 128]), B.sb("s2", [128, 8, 128])]
                v12 = [B.sb("v1", [128, 8, 16]), B.sb("v2", [128, 8, 16])]
                tmpm = B.sb("tmpm", [128, 128])
                cand = B.sb("cand", [128, 8, 256])
                sel = B.sb("sel", [128, 8, 256])
                c8a = B.sb("c8a", [128, 8, 8])
                c8b = B.sb("c8b", [128, 8, 8])
                tmp256 = B.sb("tmp256", [128, 256])
                Zt = B.sb("Zt", [128, 8])
                tm = B.sb("tm", [128, 8])
                for tt in range(4):
                    t = G * 4 + tt
                    x2c = x2r[tt % 2]
                    xk2 = ("x2r", tt % 2)
                    B.dma("sp", x2c[:], x2_d[t * 128:(t + 1) * 128, :], [("x2d", t)], [xk2])
                    B.act(hb2[:], x2c[:], AF.Square, [xk2], ["hb2", "ss"], accum=ss[:])
                    B.act(rs[:], ss[:], AF.Sqrt, ["ss", "epsb"], ["rs"], bias=epsb[:], scale=1.0 / DM)
                    B.recip(rs[:], rs[:], ["rs"], ["rs"])
                    B.stt("dve", hb2[:], x2c[:], rs[:], n2g[:], ALU.mult, ALU.mult, [xk2, "rs", "n2g"], ["hb2"])
                    for k in range(8):
                        B.tr(pT[:, k, :], hb2[:, k * 128:(k + 1) * 128], identb[:], ["hb2", "identb"], ["pT"])
                    B.cp("act", h2Tg[:, :, tt * 128:(tt + 1) * 128], pT[:], ["pT"], [("h2Tg", tt)])
                for cc in range(16):
                    p = pq[cc % 2]
                    pk = "pq%d" % (cc % 2)
                    for k in range(8):
                        B.mm(p[:], wq[:, k, cc * 128:(cc + 1) * 128], h2Tg[:, k, :], k == 0, k == 7, [("wq", k)] + h2toks, [pk])
                    B.cp("act", qTs[:, cc, :], p[:], [pk], [("qTs", cc)])
                for tt in range(4):
                    tsl = slice(tt * 128, (tt + 1) * 128)
                    for side in range(2):
                        for hh in range(2):
                            bi = (side * 2 + hh) % 2
                            p = pq[bi]
                            pk = "pq%d" % bi
                            for j in range(4):
                                h = hh * 4 + j
                                B.mm(p[:, j * 128:(j + 1) * 128], qTs[:, 2 * h + side, tsl], kT[side][:], True, True,
                                     [("qTs", 2 * h + side), ktok[side]], [pk])
                            B.cp("act", s12[side][:, hh * 4:(hh + 1) * 4, :], p[:].rearrange("p (j n) -> p j n", n=128),
                                 [pk], [("s", side, hh)])
                    for side in range(2):
                        st_ = [("s", side, 0), ("s", side, 1)]
                        vk = ("v", side)
                        for h in range(8):
                            B.max8(v12[side][:, h, 0:8], s12[side][:, h, :], st_, [vk])
                            B.mrep(tmpm[:], v12[side][:, h, 0:8], s12[side][:, h, :], -3.0e38, st_ + [vk], ["tmpm"])
                            B.max8(v12[side][:, h, 8:16], tmpm[:], ["tmpm"], [vk])
                    v1b = bass.AP(v12[0], 0, [[128, 128], [16, 8], [1, 16], [0, 16]])
                    v2b = bass.AP(v12[1], 0, [[128, 128], [16, 8], [0, 16], [1, 16]])
                    B.tt("dve", cand[:].rearrange("p h (r c) -> p h r c", c=16), v1b, v2b, ALU.add, [("v", 0), ("v", 1)], ["cand"])
                    for h in range(8):
                        B.max8(c8a[:, h, :], cand[:, h, :], ["cand"], ["c8a"])
                        B.mrep(tmp256[:], c8a[:, h, :], cand[:, h, :], -3.0e38, ["cand", "c8a"], ["tmp256"])
                        B.max8(c8b[:, h, :], tmp256[:], ["tmp256"], ["c8b"])
                    taub = bass.AP(c8b, 7, [[64, 128], [8, 8], [0, 256]])
                    mb_ = bass.AP(c8a, 0, [[64, 128], [8, 8], [0, 256]])
                    B.tt("dve", sel[:], cand[:], taub, ALU.is_ge, ["cand", "c8b"], ["sel"])
                    B.tt("dve", cand[:], cand[:], mb_, ALU.subtract, ["cand", "c8a"], ["cand"])
                    B.act(cand[:], cand[:], AF.Exp, ["cand"], ["cand"])
                    B.tt("dve", sel[:], sel[:], cand[:], ALU.mult, ["sel", "cand"], ["sel"])
                    B.red(Zt[:], sel[:], ALU.add, ["sel"], ["Zt"])
                    B.recip(Zt[:], Zt[:], ["Zt"], ["Zt"])
                    rZb = bass.AP(Zt, 0, [[8, 128], [1, 8], [0, 128]])
                    m1b = bass.AP(v12[0], 0, [[128, 128], [16, 8], [0, 128]])
                    m2b = bass.AP(v12[1], 0, [[128, 128], [16, 8], [0, 128]])
                    ek = ("eaz", tt)
                    bk = ("ebz", tt)
                    B.tt("dve", eaz[:, tt], s12[0][:], m1b, ALU.subtract, [("s", 0, 0), ("s", 0, 1), ("v", 0)], [ek])
                    B.act(eaz[:, tt], eaz[:, tt], AF.Exp, [ek], [ek])
                    B.tt("dve", eaz[:, tt], eaz[:, tt], rZb, ALU.mult, [ek, "Zt"], [ek])
                    B.tt("dve", ebz[:, tt], s12[1][:], m2b, ALU.subtract, [("s", 1, 0), ("s", 1, 1), ("v", 1)], [bk])
                    B.act(ebz[:, tt], ebz[:, tt], AF.Exp, [bk], [bk])
                    B.tt("dve", tm[:], c8b[:, :, 7], c8a[:, :, 0], ALU.subtract, ["c8a", "c8b"], ["tm"])
                    B.ts("dve", tm[:], tm[:], -6.0e-3, None, ALU.add, None, ["tm"], ["tm"])
                    B.act(tm[:], tm[:], AF.Exp, ["tm"], ["tm"])
                    B.tt("dve", thp[:, tt, :], tm[:], Zt[:], ALU.mult, ["tm", "Zt"], [("thp", tt)])
            with B.scope():
                Ebufs = [B.sb("Ebuf", [128, 16, 128], BF16) for _ in range(2)]
                Waccs = [B.sb("Wacc", [128, 2048], BF16) for _ in range(2)]
                Whs = [B.sb("Wh", [128, 2048], BF16) for _ in range(2)]
                Ub = [B.sb("Ub", [128, 8, 512], BF16) for _ in range(2)]
                Vb = [B.sb("Vb", [128, 4, 512], BF16) for _ in range(2)]
                Gb = [B.sb("Gb", [128, 512], BF16) for _ in range(2)]
                outt = [B.sb("outt", [128, DM])] * 2
                tacc = B.sb("tacc", [128, 8, 512])
                ui = 0
                vi = 0
                gbi = 0
                ei = 0
                wi = 0
                pending = []

                def build_tt(st, tt):
                    nonlocal ei, wi
                    WT = WTs[st % 2]
                    Wacc = Waccs[wi % 2]
                    wak = ("Wacc", wi % 2)
                    wi += 1
                    stages = []
                    for h in range(8):
                        Ebuf = Ebufs[ei % 2]
                        ebk = ("Ebuf", ei % 2)
                        whb = Whs[ei % 2]
                        whk = ("Wh", ei % 2)
                        ei += 1
                        Ef = Ebuf[:].rearrange("p i j -> p (i j)")
                        ea_ap = bass.AP(eaz, (tt * 8 + h) * 128 + 16 * st, [[4096, 128], [1, 16], [0, 128]])
                        eb_ap = bass.AP(ebz, (tt * 8 + h) * 128, [[4096, 128], [0, 16], [1, 128]])
                        th = thp[:, tt, h:h + 1]
                        f_e = (lambda Ebuf=Ebuf, ea_ap=ea_ap, eb_ap=eb_ap, ebk=ebk:
                               B.tt("dve", Ebuf[:], ea_ap, eb_ap, ALU.mult, [("eaz", tt), ("ebz", tt)], [ebk]))
                        if h == 0:
                            f_s = (lambda Ef=Ef, th=th, ebk=ebk:
                                   B.stt("dve", Wacc[:], Ef, th, Ef, ALU.is_ge, ALU.mult, [ebk, ("thp", tt)], [wak]))
                            f_a = None
                        else:
                            f_s = (lambda Ef=Ef, th=th, ebk=ebk, whb=whb, whk=whk:
                                   B.stt("dve", whb[:], Ef, th, Ef, ALU.is_ge, ALU.mult, [ebk, ("thp", tt)], [whk]))
                            f_a = (lambda whb=whb, whk=whk:
                                   B.tt("dve", Wacc[:], Wacc[:], whb[:], ALU.add, [wak, whk], [wak]))
                        stages.append((f_e, f_s, f_a))
                    for k_ in range(8 + 2):
                        if k_ < 8:
                            stages[k_][0]()
                        if 0 <= k_ - 1 < 8:
                            stages[k_ - 1][1]()
                        if 0 <= k_ - 2 < 8 and stages[k_ - 2][2] is not None:
                            stages[k_ - 2][2]()
                    for b2 in range(2):
                        for j in range(8):
                            ec = b2 * 8 + j
                            B.tr(pT[:, j, :], Wacc[:, ec * 128:(ec + 1) * 128], identb[:], [wak, "identb"], ["pT"])
                        B.cp("act", WT[:, b2 * 8:(b2 + 1) * 8, tt * 128:(tt + 1) * 128], pT[:], ["pT"],
                             [("WT", st % 2, ec_) for ec_ in range(b2 * 8, b2 * 8 + 8)])

                def gemm1(st):
                    nonlocal ui, gbi
                    WT = WTs[st % 2]
                    for e4 in range(4):
                        E0 = st * 16 + e4 * 4
                        ub = Ub[ui % 2]
                        uk = ("Ub", ui % 2)
                        ui += 1
                        B.dma("pool", ub[:], uTv[:, :, E0 * 128:E0 * 128 + 512], [], [uk])
                        for j in range(4):
                            ec = e4 * 4 + j
                            p = pq[ec % 2]
                            pk = "pq%d" % (ec % 2)
                            for k in range(8):
                                B.mm(p[:], ub[:, k, j * 128:(j + 1) * 128], h2Tg[:, k, :], k == 0, k == 7, [uk] + h2toks, [pk])
                            gb_ = Gb[gbi % 2]
                            gk = ("Gb", gbi % 2)
                            gbi += 1
                            B.act(gb_[:], p[:], AF.Gelu_apprx_tanh, [pk], [gk])
                            B.tt("pool", WT[:, ec, :], WT[:, ec, :], gb_[:], ALU.mult, [("WT", st % 2, ec), gk], [("WT", st % 2, ec)])

                def gemm2(st, dh):
                    nonlocal vi
                    WT = WTs[st % 2]
                    for e4 in range(4):
                        E0 = st * 16 + e4 * 4
                        vb = Vb[vi % 2]
                        vk_ = ("Vb", vi % 2)
                        vi += 1
                        B.dma("pool", vb[:], pv_d[E0 * 128:E0 * 128 + 512, dh * 512:(dh + 1) * 512].rearrange("(c p) d -> p c d", p=128),
                              [], [vk_])
                        for j in range(4):
                            ec = e4 * 4 + j
                            for dk in range(4):
                                B.mm(acc[dk][:], vb[:, j, dk * 128:(dk + 1) * 128], WT[:, ec, :], ec == 0, ec == 15,
                                     [vk_, ("WT", st % 2, ec)], ["acc%d" % dk])
                    for dk in range(4):
                        dki = dh * 4 + dk
                        if st == 0:
                            B.cp("act", oaccT[:, dki, :], acc[dk][:], ["acc%d" % dk], [("oaccT", dki)])
                        else:
                            B.cp("act", tacc[:, dki, :], acc[dk][:], ["acc%d" % dk], [("tacc", dki)])
                            pending.append(lambda dki=dki: B.tt(
                                "dve", oaccT[:, dki, :], oaccT[:, dki, :], tacc[:, dki, :], ALU.add,
                                [("tacc", dki), ("oaccT", dki)], [("oaccT", dki)]))

                for st in range(8):
                    ready = pending
                    pending = []
                    for f_ in ready:
                        f_()
                    chunks = []
                    if st > 0:
                        chunks = [lambda: gemm1(st - 1), lambda: gemm2(st - 1, 0), lambda: gemm2(st - 1, 1)]
                    for tt in range(4):
                        build_tt(st, tt)
                        if tt < len(chunks):
                            chunks[tt]()
                ready = pending
                pending = []
                for f_ in ready:
                    f_()
                gemm1(7)
                gemm2(7, 0)
                gemm2(7, 1)
                for f_ in pending:
                    f_()
                pending = []
                for tt in range(4):
                    t = G * 4 + tt
                    ot = outt[tt % 2]
                    B.dma("sp", ot[:], x2_d[t * 128:(t + 1) * 128, :], [("x2d", t)], [("outt", 0, 0), ("outt", 0, 1)])
                    for half in range(2):
                        p = pq[half]
                        pk = "pq%d" % half
                        for j in range(4):
                            dk = half * 4 + j
                            B.tr(p[:, j * 128:(j + 1) * 128], oaccT[:, dk, tt * 128:(tt + 1) * 128], ident[:],
                                 [("oaccT", dk), "ident"], [pk])
                        B.tt("dve", ot[:, half * 512:(half + 1) * 512], p[:], ot[:, half * 512:(half + 1) * 512], ALU.add,
                             [pk, ("outt", 0, half)], [("outt", 0, half)])
                    B.dma("sp", out_d[t * 128:(t + 1) * 128, :], ot[:], [("outt", 0, 0), ("outt", 0, 1)], [("outd", t)])


def finish(nc, B):
    run = B.S.emit(nc, B.root.enter_context)
    with nc.Block() as block:
        @block.sync
        def _(e):
            run("sp", e, final=True)

        @block.tensor
        def _(e):
            run("pe", e)

        @block.scalar
        def _(e):
            run("act", e)

        @block.vector
        def _(e):
            run("dve", e)

        @block.gpsimd
        def _(e):
            run("pool", e)
    B.root.close()


def host_inputs(inp, b, shared=None):
    w_in = inp["w_in"][0]
    d = {
        "x": np.ascontiguousarray(inp["x"][b]),
        "w_a": np.ascontiguousarray(w_in[:, 0:1536]),
        "norm1_g": np.ascontiguousarray(inp["norm1_g"]),
        "a_q_g": np.ascontiguousarray(inp["a_q_g"]),
        "a_k_g": np.ascontiguousarray(inp["a_k_g"]),
    }
    c = lambda a: np.ascontiguousarray(a, dtype=np.float32)
    o = 1536
    bq = [w_in[:, o + (g * 4 + r) * 64: o + (g * 4 + r + 1) * 64] for r in range(4) for g in range(2)]
    bkc, bvc, bks, bvs, bkw, bvw = [w_in[:, 2048 + i * 128: 2048 + (i + 1) * 128] for i in range(6)]
    bgate = w_in[:, 2816:2840]
    d["w_b"] = c(np.concatenate(bq + [bks, bkw, bkc, bvc, bvs, bvw, bgate], axis=1))
    d["w_m"] = c(w_in[:, 2840:4888])
    for k in ("b_q_g", "b_kc_g", "b_ks_g", "b_kw_g"):
        d[k] = c(inp[k])
    for kv in ("k", "v"):
        d[f"cmp_pos_{kv}T"] = c(np.repeat(inp[f"cmp_pos_{kv}"][0].T, 2, axis=1))
        d[f"cmp_{kv}_w1"] = c(inp[f"cmp_{kv}_w1"][0])
        d[f"cmp_{kv}_b1"] = c(inp[f"cmp_{kv}_b1"][0].reshape(2, 128).T)
        d[f"cmp_{kv}_w2"] = c(inp[f"cmp_{kv}_w2"][0])
        d[f"cmp_{kv}_b2"] = c(inp[f"cmp_{kv}_b2"])
    d["b_merge"] = c(inp["b_merge"][0].reshape(1, 2048))
    d["w_up_a"] = c(inp["w_up_a"][0])
    d["w_up_b"] = c(inp["w_up_b"][0])
    d["w_out"] = c(inp["w_out"][0])
    d["norm2_g"] = c(inp["norm2_g"])
    d["peer_wq"] = c(inp["peer_wq"][0])
    d["peer_k1T"] = c(inp["peer_k1"][0].T)
    d["peer_k2T"] = c(inp["peer_k2"][0].T)
    d["peer_uT"] = shared["uT"] if shared is not None else c(inp["peer_u"][0].T)
    d["peer_v"] = shared["v"] if shared is not None else c(inp["peer_v"][0])
    return d


def kernel(**inputs):
    nc, B = build_program()
    finish(nc, B)
    shared = {"uT": np.ascontiguousarray(inputs["peer_u"][0].T, dtype=np.float32),
              "v": np.ascontiguousarray(inputs["peer_v"][0], dtype=np.float32)}
    in_maps = [host_inputs(inputs, b, shared) for b in range(8)]
    res = run_bass_kernel_spmd(nc, in_maps, core_ids=list(range(8)))
    return np.stack([np.asarray(r["out"]) for r in res.results], 0).astype(np.float32)
```
